# Optimizing a Trainium2 kernel written in Bass

```python
import jax
import jax.numpy as jnp
from jax import lax
import numpy as np

D_MODEL = 2048
BATCH = 2
SEQ = 4096
DEPTH = 2

GRID_W = 64
CTX_LEN = 256

ATT_HEADS = 8
ATT_KV_HEADS = 4
HEAD_DIM = 128
ATT_GROUP = ATT_HEADS // ATT_KV_HEADS
WINDOW = 128
ATT_BLOCK = 128
ROPE_BASE = 10000.0

HG_HEADS = 8
HG_KEY = 128
HG_VAL = (D_MODEL // 2) // HG_HEADS
HG_CHUNK = 64
NORM_EPS = 1e-6

ATT_Q_W = ATT_HEADS * HEAD_DIM
ATT_KV_W = ATT_KV_HEADS * HEAD_DIM
HG_K_W = HG_HEADS * HG_KEY
HG_V_W = HG_HEADS * HG_VAL
MIX_COLS = ('att_q', 'att_k', 'att_v', 'hg_q', 'hg_f_fwd', 'hg_f_bwd', 'hg_i', 'hg_g')
MIX_WIDTHS = (ATT_Q_W, ATT_KV_W, ATT_KV_W, HG_K_W, HG_K_W, HG_K_W, HG_V_W, HG_V_W)
CTX_SIDE_COLS = ('att_k', 'att_v', 'hg_f_fwd', 'hg_f_bwd', 'hg_i')
MIX_IN_W = ATT_Q_W + 2 * ATT_KV_W + 3 * HG_K_W + 2 * HG_V_W
MIX_OUT_IN = ATT_Q_W + HG_V_W

POOL_WINDOWS = (2, 4, 8, 16)
POOL_GROUPS = 4
POOL_CH = D_MODEL // POOL_GROUPS

MOE_GROUPS = 4
MOE_EXPERTS_PER_GROUP = 8
N_EXPERTS = MOE_GROUPS * MOE_EXPERTS_PER_GROUP
MOE_TOP_K = 2
EXPERT_FF = D_MODEL // 4
MOE_BLOCK = 128

LN_EPS = 1e-5
DEEPNORM_ALPHA = (2 * DEPTH) ** 0.25
DEEPNORM_BETA = (8 * DEPTH) ** -0.25

kernel_name = 'hybrid_swa_hgrn2_pool_hmoe_dit'


def _layer_norm(x, g, b):
    xf = x.astype(jnp.float32)
    mu = jnp.mean(xf, axis=-1, keepdims=True)
    var = jnp.mean(jnp.square(xf - mu), axis=-1, keepdims=True)
    y = (xf - mu) * lax.rsqrt(var + LN_EPS) * g.astype(jnp.float32) + b.astype(jnp.float32)
    return y.astype(x.dtype)


def _split_heads(t, n_heads):
    return t.reshape(t.shape[0], t.shape[1], n_heads, -1)


def _project(h, w_in, names):
    starts = dict(zip(MIX_COLS, np.cumsum((0,) + MIX_WIDTHS[:-1]).tolist()))
    widths = dict(zip(MIX_COLS, MIX_WIDTHS))
    if tuple(names) == MIX_COLS:
        w = w_in
    else:
        w = jnp.concatenate([w_in[:, starts[n]:starts[n] + widths[n]] for n in names], axis=1)
    p = h @ w
    cuts = np.cumsum([widths[n] for n in names])[:-1].tolist()
    return dict(zip(names, jnp.split(p, cuts, axis=-1)))


def _axial_rope(t, rows):
    n = t.shape[1]
    half = HEAD_DIM // 2
    n_freq = half // 2
    row = jnp.repeat(jnp.arange(rows), GRID_W).astype(jnp.float32)
    col = (jnp.arange(n) % GRID_W).astype(jnp.float32)
    inv_freq = ROPE_BASE ** (-jnp.arange(n_freq, dtype=jnp.float32) / n_freq)

    def rotate(seg, pos):
        ang = pos[:, None] * inv_freq[None, :]
        cos = jnp.cos(ang)[None, :, None, :]
        sin = jnp.sin(ang)[None, :, None, :]
        a, b = seg[..., :n_freq], seg[..., n_freq:]
        return jnp.concatenate([a * cos - b * sin, b * cos + a * sin], axis=-1)

    tf = t.astype(jnp.float32)
    out = jnp.concatenate([rotate(tf[..., :half], row), rotate(tf[..., half:], col)], axis=-1)
    return out.astype(t.dtype)


def _window_attention(q, k, v, k_ctx, v_ctx, sink):
    b, n = q.shape[0], q.shape[1]
    nb = n // ATT_BLOCK
    n_ctx = k_ctx.shape[1]
    scale = HEAD_DIM ** -0.5
    qb = q.reshape(b, nb, ATT_BLOCK, ATT_KV_HEADS, ATT_GROUP, HEAD_DIM)

    def band(t):
        tp = jnp.pad(t, ((0, 0), (ATT_BLOCK, ATT_BLOCK), (0, 0), (0, 0)))
        tp = tp.reshape(b, nb + 2, ATT_BLOCK, ATT_KV_HEADS, HEAD_DIM)
        return jnp.concatenate([tp[:, :-2], tp[:, 1:-1], tp[:, 2:]], axis=2)

    kb, vb = band(k), band(v)
    q_pos = jnp.arange(n).reshape(nb, ATT_BLOCK)
    k_pos = (jnp.arange(nb) * ATT_BLOCK - ATT_BLOCK)[:, None] + jnp.arange(3 * ATT_BLOCK)[None, :]
    valid = ((jnp.abs(q_pos[:, :, None] - k_pos[:, None, :]) <= WINDOW)
             & (k_pos >= 0)[:, None, :] & (k_pos < n)[:, None, :])
    s_loc = jnp.einsum('bnqhgd,bnkhd->bnhgqk', qb, kb).astype(jnp.float32) * scale
    s_loc = jnp.where(valid[None, :, None, None], s_loc, -jnp.inf)
    s_ctx = jnp.einsum('bnqhgd,bmhd->bnhgqm', qb, k_ctx).astype(jnp.float32) * scale
    s_sink = jnp.broadcast_to(sink.astype(jnp.float32).reshape(1, 1, ATT_KV_HEADS, ATT_GROUP, 1, 1),
                              s_loc.shape[:-1] + (1,))
    p = jax.nn.softmax(jnp.concatenate([s_loc, s_ctx, s_sink], axis=-1), axis=-1).astype(v.dtype)
    nk = 3 * ATT_BLOCK
    o = (jnp.einsum('bnhgqk,bnkhd->bnqhgd', p[..., :nk], vb)
         + jnp.einsum('bnhgqm,bmhd->bnqhgd', p[..., nk:nk + n_ctx], v_ctx))
    return o.reshape(b, n, ATT_HEADS * HEAD_DIM)


def _context_attention(q, k, v, sink):
    b, n_ctx = q.shape[0], q.shape[1]
    qg = q.reshape(b, n_ctx, ATT_KV_HEADS, ATT_GROUP, HEAD_DIM)
    s = jnp.einsum('blhgd,bmhd->bhglm', qg, k).astype(jnp.float32) * HEAD_DIM ** -0.5
    s_sink = jnp.broadcast_to(sink.astype(jnp.float32).reshape(1, ATT_KV_HEADS, ATT_GROUP, 1, 1),
                              s.shape[:-1] + (1,))
    p = jax.nn.softmax(jnp.concatenate([s, s_sink], axis=-1), axis=-1)[..., :n_ctx].astype(v.dtype)
    o = jnp.einsum('bhglm,bmhd->blhgd', p, v)
    return o.reshape(b, n_ctx, ATT_HEADS * HEAD_DIM)


def _hgrn2_gates(f_logit, lb):
    lbh = lb.reshape(HG_HEADS, HG_KEY)
    f = lbh + (1.0 - lbh) * jax.nn.sigmoid(_split_heads(f_logit, HG_HEADS).astype(jnp.float32))
    return 1.0 - f, jnp.log(f)


def _gla_chunked(q, k, v, log_f, s0):
    b, n, h, _ = q.shape
    n_val = v.shape[-1]
    nc = n // HG_CHUNK

    def chunks(t):
        return t.astype(jnp.float32).reshape(b, nc, HG_CHUNK, h, t.shape[-1]).transpose(1, 0, 3, 2, 4)

    incl = jnp.tril(jnp.ones((HG_CHUNK, HG_CHUNK), dtype=bool))[:, :, None]

    def step(state, inp):
        qi, ki, vi, gi = inp
        cum = jnp.cumsum(gi, axis=2)
        cum_end = cum[:, :, -1]
        o_inter = jnp.einsum('bhtk,bhkv->bhtv', qi * jnp.exp(cum), state)
        rel = jnp.exp(jnp.where(incl, cum[:, :, :, None, :] - cum[:, :, None, :, :], -jnp.inf))
        scores = jnp.einsum('bhtk,bhsk,bhtsk->bhts', qi, ki, rel)
        o = o_inter + jnp.einsum('bhts,bhsv->bhtv', scores, vi)
        new_state = (jnp.exp(cum_end)[..., None] * state
                     + jnp.einsum('bhsk,bhsv->bhkv', ki * jnp.exp(cum_end[:, :, None] - cum), vi))
        return new_state, o

    s_fin, o = lax.scan(step, s0.astype(jnp.float32), (chunks(q), chunks(k), chunks(v), chunks(log_f)))
    return o.transpose(1, 0, 3, 2, 4).reshape(b, n, h, n_val), s_fin


def _gla_final_state(k, v, log_f):
    g = log_f.astype(jnp.float32)
    to_end = jnp.flip(jnp.cumsum(jnp.flip(g, axis=1), axis=1), axis=1) - g
    return jnp.einsum('bnhk,bnhv->bhkv', k.astype(jnp.float32) * jnp.exp(to_end), v.astype(jnp.float32))


def _hgrn2_bidir(px, pc, lb_fwd, lb_bwd, ctx_out):
    b = px['hg_i'].shape[0]
    q_x = jax.nn.silu(_split_heads(px['hg_q'], HG_HEADS).astype(jnp.float32))
    v_x = _split_heads(px['hg_i'], HG_HEADS)
    v_c = _split_heads(pc['hg_i'], HG_HEADS)
    q_c = jax.nn.silu(_split_heads(pc['hg_q'], HG_HEADS).astype(jnp.float32)) if ctx_out else None
    o_x, o_c = 0.0, 0.0
    for f_name, lb, reverse in (('hg_f_fwd', lb_fwd, False), ('hg_f_bwd', lb_bwd, True)):
        seq = (lambda t: jnp.flip(t, axis=1)) if reverse else (lambda t: t)
        k_x, g_x = _hgrn2_gates(px[f_name], lb)
        k_c, g_c = _hgrn2_gates(pc[f_name], lb)
        if ctx_out:
            s0 = jnp.zeros((b, HG_HEADS, HG_KEY, HG_VAL), jnp.float32)
            oc, s_ctx = _gla_chunked(seq(q_c), seq(k_c), seq(v_c), seq(g_c), s0)
            o_c = o_c + seq(oc)
        else:
            s_ctx = _gla_final_state(seq(k_c), seq(v_c), seq(g_c))
        ox, _ = _gla_chunked(seq(q_x), seq(k_x), seq(v_x), seq(g_x), s_ctx)
        o_x = o_x + seq(ox)
    return o_x, (o_c if ctx_out else None)


def _gated_rmsnorm(o, gate, gain):
    o = o * lax.rsqrt(jnp.mean(jnp.square(o), axis=-1, keepdims=True) + NORM_EPS)
    o = o.reshape(o.shape[0], o.shape[1], HG_V_W) * gain.astype(jnp.float32)
    return (o * jax.nn.silu(gate.astype(jnp.float32))).astype(gate.dtype)


def _even_mixer(hx, hc, rows, w_in, sink, lb_fwd, lb_bwd, norm_g, w_out, ctx_out):
    px = _project(hx, w_in, MIX_COLS)
    pc = _project(hc, w_in, MIX_COLS if ctx_out else CTX_SIDE_COLS)
    q_x = _axial_rope(_split_heads(px['att_q'], ATT_HEADS), rows)
    k_x = _axial_rope(_split_heads(px['att_k'], ATT_KV_HEADS), rows)
    v_x = _split_heads(px['att_v'], ATT_KV_HEADS)
    k_c = _split_heads(pc['att_k'], ATT_KV_HEADS)
    v_c = _split_heads(pc['att_v'], ATT_KV_HEADS)
    att_x = _window_attention(q_x, k_x, v_x, k_c, v_c, sink)
    o_x, o_c = _hgrn2_bidir(px, pc, lb_fwd, lb_bwd, ctx_out)
    hg_x = _gated_rmsnorm(o_x, px['hg_g'], norm_g)
    y_x = jnp.concatenate([att_x, hg_x.astype(att_x.dtype)], axis=-1) @ w_out
    y_c = None
    if ctx_out:
        att_c = _context_attention(_split_heads(pc['att_q'], ATT_HEADS), k_c, v_c, sink)
        hg_c = _gated_rmsnorm(o_c, pc['hg_g'], norm_g)
        y_c = jnp.concatenate([att_c, hg_c.astype(att_c.dtype)], axis=-1) @ w_out
    return y_x, y_c


def _pool_mixer(h, w_in, w_grp, scale, w_out):
    b, n, _ = h.shape
    u = (h @ w_in).reshape(b, n, POOL_GROUPS, POOL_CH)
    uf = u.astype(jnp.float32)
    pref = jnp.concatenate([jnp.zeros((b, 1, POOL_GROUPS, POOL_CH), jnp.float32),
                            jnp.cumsum(uf, axis=1)], axis=1)
    t = jnp.arange(n)
    means = []
    for gi, w in enumerate(POOL_WINDOWS):
        lo = jnp.clip(t - w // 2, 0, n)
        hi = jnp.clip(t + w - w // 2, 0, n)
        pg = pref[:, :, gi]
        means.append((pg[:, hi] - pg[:, lo]) / (hi - lo).astype(jnp.float32)[None, :, None])
    mixed = (jnp.stack(means, axis=2) - uf).astype(h.dtype)
    y = jnp.einsum('bngc,gcd->bngd', mixed, w_grp).reshape(b, n, D_MODEL) * scale
    return y @ w_out


def _hier_moe(h, w_rg, b_rg, w_re, b_re, w1, w3, w2):
    n_tok, d = h.shape
    g_prob = jax.nn.softmax((h @ w_rg + b_rg).astype(jnp.float32), axis=-1)
    g_val, g_idx = lax.top_k(g_prob, 1)
    e_logits = (h @ w_re + b_re).astype(jnp.float32).reshape(n_tok, MOE_GROUPS, MOE_EXPERTS_PER_GROUP)
    e_logits = jnp.take_along_axis(e_logits, g_idx[:, :, None], axis=1)[:, 0]
    e_val, e_idx = lax.top_k(jax.nn.softmax(e_logits, axis=-1), MOE_TOP_K)
    gate = g_val * e_val / jnp.sum(e_val, axis=-1, keepdims=True)
    expert = g_idx * MOE_EXPERTS_PER_GROUP + e_idx

    n_assign = n_tok * MOE_TOP_K
    flat_e = expert.reshape(n_assign)
    order = jnp.argsort(flat_e)
    e_sorted = flat_e[order]
    counts = jnp.bincount(flat_e, length=N_EXPERTS)
    padded = (counts + MOE_BLOCK - 1) // MOE_BLOCK * MOE_BLOCK
    start = jnp.cumsum(counts) - counts
    pad_end = jnp.cumsum(padded)
    pad_start = pad_end - padded
    dest = pad_start[e_sorted] + jnp.arange(n_assign) - start[e_sorted]
    n_blocks = -(-(n_assign + N_EXPERTS * (MOE_BLOCK - 1)) // MOE_BLOCK)
    n_slots = n_blocks * MOE_BLOCK
    slot_tok = jnp.full((n_slots,), n_tok, jnp.int32).at[dest].set((order // MOE_TOP_K).astype(jnp.int32))
    slot_gate = jnp.zeros((n_slots,), h.dtype).at[dest].set(gate.reshape(n_assign)[order].astype(h.dtype))
    block_expert = jnp.minimum(jnp.searchsorted(pad_end, jnp.arange(n_blocks) * MOE_BLOCK, side='right'),
                               N_EXPERTS - 1)
    h_pad = jnp.concatenate([h, jnp.zeros((1, d), h.dtype)], axis=0)
    xb = h_pad[slot_tok].reshape(n_blocks, MOE_BLOCK, d)

    def expert_block(args):
        xe, e = args
        return (jax.nn.silu(xe @ w1[e]) * (xe @ w3[e])) @ w2[e]

    yb = lax.map(expert_block, (xb, block_expert)).reshape(n_slots, d)
    y = jnp.zeros((n_tok + 1, d), h.dtype).at[slot_tok].add(yb * slot_gate[:, None])
    return y[:n_tok]


def setup_inputs(seed: int = 0) -> dict:
    key = jax.random.key(seed)
    ks = jax.random.split(key, 24)
    d = D_MODEL
    n_even = (DEPTH + 1) // 2
    n_odd = DEPTH // 2

    def nrm(k, shape, s):
        return jax.random.normal(k, shape, jnp.float32) * s

    return {
        'x': nrm(ks[0], (BATCH, SEQ, d), 1.0),
        'c': nrm(ks[1], (BATCH, d), 1.0),
        'ctx': nrm(ks[2], (BATCH, CTX_LEN, d), 1.0),
        'c_ctx': nrm(ks[3], (d,), 1.0),
        'ada_w': nrm(ks[4], (DEPTH, d, 6 * d), 0.5 * d ** -0.5),
        'ada_b': nrm(ks[5], (DEPTH, 6 * d), 0.02),
        'ln_g': 1.0 + nrm(ks[6], (DEPTH, 2, d), 0.02),
        'ln_b': nrm(ks[7], (DEPTH, 2, d), 0.02),
        'mix_w_in': nrm(ks[8], (n_even, d, MIX_IN_W), d ** -0.5),
        'att_sink': nrm(ks[9], (n_even, ATT_HEADS), 0.5),
        'hg_lb': nrm(ks[10], (2, DEPTH + 1, HG_K_W), 0.1),
        'hg_norm_g': 1.0 + nrm(ks[11], (n_even, HG_V_W), 0.02),
        'mix_w_out': nrm(ks[12], (n_even, MIX_OUT_IN, d), MIX_OUT_IN ** -0.5 * DEEPNORM_BETA),
        'pool_w_in': nrm(ks[13], (n_odd, d, d), d ** -0.5),
        'pool_w_grp': nrm(ks[14], (n_odd, POOL_GROUPS, POOL_CH, POOL_CH), POOL_CH ** -0.5),
        'pool_scale': 1.0 + nrm(ks[15], (n_odd, d), 0.1),
        'pool_w_out': nrm(ks[16], (n_odd, d, d), d ** -0.5 * DEEPNORM_BETA),
        'rt_group_w': nrm(ks[17], (DEPTH, d, MOE_GROUPS), d ** -0.5),
        'rt_group_b': nrm(ks[18], (DEPTH, MOE_GROUPS), 0.01),
        'rt_expert_w': nrm(ks[19], (DEPTH, d, N_EXPERTS), d ** -0.5),
        'rt_expert_b': nrm(ks[20], (DEPTH, N_EXPERTS), 0.01),
        'moe_w1': nrm(ks[21], (DEPTH, N_EXPERTS, d, EXPERT_FF), d ** -0.5),
        'moe_w3': nrm(ks[22], (DEPTH, N_EXPERTS, d, EXPERT_FF), d ** -0.5),
        'moe_w2': nrm(ks[23], (DEPTH, N_EXPERTS, EXPERT_FF, d), EXPERT_FF ** -0.5 * DEEPNORM_BETA),
    }


def reference(x, c, ctx, c_ctx, ada_w, ada_b, ln_g, ln_b, mix_w_in, att_sink, hg_lb, hg_norm_g,
              mix_w_out, pool_w_in, pool_w_grp, pool_scale, pool_w_out, rt_group_w, rt_group_b,
              rt_expert_w, rt_expert_b, moe_w1, moe_w3, moe_w2):
    b, n, d = x.shape
    n_ctx = ctx.shape[1]
    rows = n // GRID_W
    lower_bounds = jnp.cumsum(jax.nn.softmax(hg_lb.astype(jnp.float32), axis=1), axis=1)
    silu_c = jax.nn.silu(c)
    silu_cc = jax.nn.silu(c_ctx)
    for l in range(DEPTH):
        even = l % 2 == 0
        ctx_next = any(j % 2 == 0 for j in range(l + 1, DEPTH))
        mod = (silu_c @ ada_w[l] + ada_b[l])[:, None, :]
        sh1, sc1, g1, sh2, sc2, g2 = jnp.split(mod, 6, axis=-1)
        hx = x * (1 + sc1) + sh1
        if even or ctx_next:
            n_cols = 6 * d if ctx_next else 2 * d
            mc = jnp.split(silu_cc @ ada_w[l][:, :n_cols] + ada_b[l][:n_cols], n_cols // d)
            hc = ctx * (1 + mc[1]) + mc[0]
        if even:
            e = l // 2
            y_x, y_c = _even_mixer(hx, hc, rows, mix_w_in[e], att_sink[e], lower_bounds[0, l],
                                   lower_bounds[1, l], hg_norm_g[e], mix_w_out[e], ctx_next)
        else:
            o = l // 2
            y_x = _pool_mixer(hx, pool_w_in[o], pool_w_grp[o], pool_scale[o], pool_w_out[o])
            y_c = _pool_mixer(hc, pool_w_in[o], pool_w_grp[o], pool_scale[o], pool_w_out[o]) if ctx_next else None
        x = _layer_norm(DEEPNORM_ALPHA * x + g1 * y_x, ln_g[l, 0], ln_b[l, 0])
        tokens = (x * (1 + sc2) + sh2).reshape(b * n, d)
        if ctx_next:
            ctx = _layer_norm(DEEPNORM_ALPHA * ctx + mc[2] * y_c, ln_g[l, 0], ln_b[l, 0])
            tokens = jnp.concatenate([tokens, (ctx * (1 + mc[4]) + mc[3]).reshape(b * n_ctx, d)], axis=0)
        y = _hier_moe(tokens, rt_group_w[l], rt_group_b[l], rt_expert_w[l], rt_expert_b[l],
                      moe_w1[l], moe_w3[l], moe_w2[l])
        x = _layer_norm(DEEPNORM_ALPHA * x + g2 * y[:b * n].reshape(b, n, d), ln_g[l, 1], ln_b[l, 1])
        if ctx_next:
            ctx = _layer_norm(DEEPNORM_ALPHA * ctx + mc[5] * y[b * n:].reshape(b, n_ctx, d),
                              ln_g[l, 1], ln_b[l, 1])
    return x
```

```python
import numpy as np
import concourse.bass as bass
import concourse.mybir as mybir
from concourse.bass_utils import run_bass_kernel_spmd

F32 = mybir.dt.float32
BF16 = mybir.dt.bfloat16
ALU = mybir.AluOpType
AF = mybir.ActivationFunctionType
AX = mybir.AxisListType

D = 2048
NCORE = 8
T = 1024
TE = 1280
NCTX = 256
TA = TE + NCTX
KC = 16
GRID_W = 64
SEQ = 4096
ALPHA = 4.0 ** 0.25
LN_EPS = 1e-5
NORM_EPS = 1e-6
ATT_SCALE = 128.0 ** -0.5
NEXP = 32
FF = 512


import types


def _freeze(fn):
    if getattr(fn, "__closure__", None) is None:
        return fn
    cells = []
    for c in fn.__closure__:
        try:
            cells.append(types.CellType(c.cell_contents))
        except ValueError:
            cells.append(c)
    g = types.FunctionType(fn.__code__, fn.__globals__, fn.__name__, fn.__defaults__, tuple(cells))
    g.__kwdefaults__ = fn.__kwdefaults__
    return g


class FW:
    ENG = ("pe", "act", "dve", "pool", "sp")
    NDMA = 24

    def __init__(self, nc):
        self.nc = nc
        self.ops = []
        self.last_w = {}
        self.readers = {}
        self.pending = {e: set() for e in self.ENG}
        self.dma_prev = {}
        self.n_dma = {}

    def _record(self, engine, fn, reads, writes, kind):
        deps = set(self.pending[engine])
        self.pending[engine] = set()
        for k in reads:
            w = self.last_w.get(k)
            if w is not None:
                deps.add(w)
        for k in writes:
            w = self.last_w.get(k)
            if w is not None:
                deps.add(w)
            for r in self.readers.get(k, ()):
                deps.add(r)
        oid = len(self.ops)
        op = dict(engine=engine, fn=_freeze(fn), deps=deps, kind=kind, marked=False, slot=None)
        if kind == "dma":
            k_ = self.n_dma.get(engine, 0)
            self.n_dma[engine] = k_ + 1
            slot = (engine, k_ % self.NDMA)
            prev = self.dma_prev.get(slot)
            if prev is not None:
                deps.add(prev)
            self.dma_prev[slot] = oid
            op["slot"] = slot
        self.ops.append(op)
        for k in reads:
            self.readers.setdefault(k, []).append(oid)
        for k in writes:
            self.last_w[k] = oid
            self.readers[k] = []
        return oid

    def op(self, engine, fn, reads=(), writes=()):
        return self._record(engine, fn, list(reads), list(writes), "c")

    def dma(self, engine, fn, reads=(), writes=()):
        return self._record(engine, fn, list(reads), list(writes), "dma")

    def cc(self, fn, reads=(), writes=()):
        return self._record("pool", fn, list(reads), list(writes), "cc")

    def barrier(self):
        allops = set()
        last = {}
        for i, o in enumerate(self.ops):
            if o["kind"] in ("dma", "cc"):
                allops.add(i)
            else:
                last[o["engine"]] = i
        allops |= set(last.values())
        for e in self.ENG:
            self.pending[e] |= allops

    def emit(self, nsem_ctx):
        nc = self.nc
        ops = self.ops
        for o in ops:
            for d in o["deps"]:
                ops[d]["marked"] = True
        for e in self.ENG:
            for d in self.pending[e]:
                ops[d]["marked"] = True
        cnt = {e: 0 for e in self.ENG}
        dcnt = {}
        ncc = 0
        for o in ops:
            if o["kind"] == "dma":
                s = o["slot"]
                dcnt[s] = dcnt.get(s, 0) + 16
                o["ev"] = (("d", s), dcnt[s])
            elif o["kind"] == "cc":
                o["ev"] = (("c", ncc), 1)
                ncc += 1
            elif o["marked"]:
                cnt[o["engine"]] += 1
                o["ev"] = (("e", o["engine"]), cnt[o["engine"]])
        sems = {}

        def sem(key):
            if key not in sems:
                sems[key] = nsem_ctx.enter_context(nc.semaphore("s_" + "_".join(str(x) for x in (key[1] if isinstance(key[1], tuple) else (key[1],))) + "_" + key[0]))
            return sems[key]

        for e in self.ENG:
            sem(("e", e))
        for s in sorted(dcnt):
            sem(("d", s))
        for c in range(ncc):
            sem(("c", c))
        streams = {e: [] for e in self.ENG}
        for i, o in enumerate(ops):
            streams[o["engine"]].append(i)
        final = {e: set(self.pending[e]) for e in self.ENG}

        def run(engine, eng):
            known = {}

            def wait_for(d):
                od = ops[d]
                if od["kind"] == "c" and od["engine"] == engine and engine in ("pe", "sp"):
                    return
                key, val = od["ev"]
                if known.get(key, 0) < val:
                    eng.wait_ge(sem(key), val)
                    known[key] = val

            for i in streams[engine]:
                o = ops[i]
                for d in sorted(o["deps"]):
                    wait_for(d)
                ins = o["fn"](eng)
                if o["kind"] in ("dma", "cc") or o["marked"]:
                    key, val = o["ev"]
                    if o["kind"] == "dma":
                        ins.then_inc(sem(key), 16)
                    else:
                        ins.then_inc(sem(key), 1)
            for d in sorted(final[engine]):
                wait_for(d)

        with nc.Block() as block:
            @block.sync
            def _(e):
                run("sp", e)

            @block.scalar
            def _(e):
                run("act", e)

            @block.vector
            def _(e):
                run("dve", e)

            @block.tensor
            def _(e):
                run("pe", e)

            @block.gpsimd
            def _(e):
                run("pool", e)


def build(cfg):
    import contextlib
    NSEG = cfg.get("nseg", 4)
    SEQL = NSEG * 1024
    NG = cfg.get("ng", 4)
    EPG = cfg.get("epg", 8)
    NE = NG * EPG
    NR = NG + NE
    upto = cfg.get("upto", 99)
    nc = bass.Bass("TRN2", target_bir_lowering=False)
    fw = FW(nc)
    es = contextlib.ExitStack()

    def din(name, shape, dt=F32):
        return nc.dram_tensor(name, list(shape), dt, kind="ExternalInput").ap()

    def dint(name, shape, dt=F32):
        return nc.dram_tensor(name, list(shape), dt).ap()

    sbn = {"n": 0}

    def sb(name, shape, dt=F32, stack=es):
        sbn["n"] += 1
        return stack.enter_context(nc.sbuf_tensor("sb%d_%s" % (sbn["n"], name), list(shape), dt))

    def ld(eng, out, in_, reads=(), writes=()):
        return fw.dma(eng, lambda e: e.dma_start(out=out, in_=in_), reads=reads, writes=writes)

    x_in = din("x", [SEQL, D])
    ctx_in = din("ctx", [NCTX, D])
    cvec = din("cvec", [128, KC * 2])
    ada_w = din("ada_w", [2, D, 6 * D])
    ada_b = din("ada_b", [128, 2 * 96])
    ln_g = din("ln_g", [4, D])
    ln_b = din("ln_b", [4, D])
    ident_in = din("ident", [128, 128])
    ropeR_in = din("ropeR", [128, 128])
    cos_in = din("rcos", [128, SEQL + 256])
    sin_in = din("rsin", [128, SEQL + 256])
    amask_in = din("amask", [128, 2 * 128])
    hmask_in = din("hmask", [64, 2 * 512])
    resetp_in = din("resetp", [128, 1024])
    win_in = din("mix_w_in", [D, 7168])
    sink_in = din("att_sink", [1, 8])
    hglb_in = din("hg_lb", [128, 2 * 3 * 8])
    hgng_in = din("hg_norm_g", [128, 8])
    wout_in = din("mix_w_out", [D, D])
    pwin_in = din("pool_w_in", [D, D])
    pwgrp_in = din("pool_w_grp", [4, 512, 512])
    pscale_in = din("pool_scale", [128, 16])
    pwout_in = din("pool_w_out", [D, D])
    pinv_in = din("pinv", [1, 4 * SEQL])
    rtw_in = din("rt_w", [2, D, NR])
    rtb_in = din("rt_b", [2, NR])
    w1_in = din("moe_w1", [2, NE, D, FF])
    w3_in = din("moe_w3", [2, NE, D, FF])
    w2_in = din("moe_w2", [2, NE, FF, D])
    out_d = nc.dram_tensor("out", [SEQL, D], F32, kind="ExternalOutput").ap()
    dbg = None
    if "dbg_shape" in cfg:
        dbg = nc.dram_tensor("dbg", list(cfg["dbg_shape"]), cfg.get("dbg_dt", F32), kind="ExternalOutput").ap()

    grow = [[dint("grow%d%d" % (l, w), [1, D]) for w in range(2)] for l in range(2)]
    att_d = [dint("att_d%d" % s, [128, 8 * 1024], BF16) for s in range(NSEG)]
    qe_d = [[dint("qe_d%d%d" % (d, s), [128, 8 * 1024], BF16) for s in range(NSEG)] for d in range(2)]
    ke_d = [[dint("ke_d%d%d" % (d, s), [128, 8 * 1024], BF16) for s in range(NSEG)] for d in range(2)]
    dec_d = [[dint("dec_d%d%d" % (d, s), [128, 8 * 16]) for s in range(NSEG)] for d in range(2)]
    vi_d = [dint("vi_d%d" % s, [64, 16 * 1024], BF16) for s in range(NSEG)]
    g_d = [dint("g_d%d" % s, [128, 8 * 1024], BF16) for s in range(NSEG)]
    of_d = [dint("of_d%d" % s, [128, 8 * 1024], BF16) for s in range(NSEG)]
    hg_d = [dint("hg_d%d" % s, [128, 8 * 1024], BF16) for s in range(NSEG)]
    x1_d = [dint("x1_d%d" % l, [SEQL, D]) for l in range(2)]
    xl0_d = dint("xl0_d", [SEQL, D])

    ps = es.enter_context(nc.psum_tensor("ps", [128, 4096], F32))

    def bank(b, n=512, o=0):
        return ps[:, b * 512 + o:b * 512 + o + n]

    ident_f = sb("ident_f", [128, 128])
    ident_b = sb("ident_b", [128, 128], BF16)
    ones_b = sb("ones_b", [128, 128], BF16)
    ropeR_b = sb("ropeR_b", [128, 128], BF16)
    amask_b = sb("amask_b", [128, 2, 128], BF16)
    hmask_b = sb("hmask_b", [64, 2, 512], BF16)
    resetp = sb("resetp", [128, 1024])
    modT = sb("modT", [128, 2, 96, 2])
    mod1 = sb("mod1", [128, 2, 96, 2])
    esink = sb("esink", [128, 8])
    lbt = sb("lbt", [128, 2, 8])
    omlb = sb("omlb", [128, 2, 8])
    normg = sb("normg", [128, 8])
    pscale = sb("pscale", [128, 16])
    kcT = sb("kcT", [128, 4, NCTX], BF16)
    vc_tok = sb("vc_tok", [128, 2, 512], BF16)
    S0 = [sb("S0_%d" % d, [128, 8, 128]) for d in range(2)]

    ld("sp", ident_f[:, :], ident_in, writes=["ident_f"])
    ld("pool", ident_b[:, :], ident_in, writes=["ident_b"])
    ld("pool", ropeR_b[:, :], ropeR_in, writes=["ropeR_b"])
    ld("pool", amask_b[:, :, :], amask_in.rearrange("p (w t) -> p w t", w=2), writes=["amask_b"])
    ld("pool", hmask_b[:, :, :], hmask_in.rearrange("p (w t) -> p w t", w=2), writes=["hmask_b"])
    ld("sp", resetp[:, :], resetp_in, writes=["resetp"])
    ld("sp", normg[:, :], hgng_in, writes=["normg"])
    ld("sp", pscale[:, :], pscale_in, writes=["pscale"])
    fw.op("dve", lambda e: e.memset(ones_b[:, :], 1.0), writes=["ones_b"])

    PSK = [("ps", b) for b in range(8)]
    rot = {"n": 0}

    def mm_group(out_ap, pairs, reads, writes):
        def f(e):
            ins = None
            n = len(pairs)
            for i, (l, r) in enumerate(pairs):
                ins = e.matmul(out_ap, lhsT=l, rhs=r, start=(i == 0), stop=(i == n - 1))
            return ins
        return fw.op("pe", f, reads=reads, writes=writes)

    with contextlib.ExitStack() as p0:
        cv = sb("cv", [128, KC, 2], stack=p0)
        scv = sb("scv", [128, KC, 2], stack=p0)
        adab = sb("adab", [128, 2, 96], stack=p0)
        wt = [sb("adaw%d" % i, [128, KC, 384], stack=p0) for i in range(2)]
        sk = sb("sk", [128, 8], stack=p0)
        lb3 = sb("lb3", [128, 2, 3, 8], stack=p0)
        lbs = sb("lbs", [128, 2, 8], stack=p0)
        tr16 = sb("tr16", [128, 16], stack=p0)
        tr16o = sb("tr16o", [16, 128], stack=p0)
        ld("sp", cv[:, :, :], cvec.rearrange("p (k v) -> p k v", v=2), writes=["cv"])
        ld("sp", adab[:, :, :], ada_b.rearrange("p (l c) -> p l c", l=2), writes=["adab"])
        ld("sp", sk[:, :], sink_in.partition_broadcast(128), writes=["sk"])
        ld("sp", lb3[:, :, :, :], hglb_in.rearrange("p (d l h) -> p d l h", d=2, l=3), writes=["lb3"])
        fw.op("act", lambda e: e.activation(out=scv[:, :, :], in_=cv[:, :, :], func=AF.Silu), reads=["cv"], writes=["scv"])
        fw.op("act", lambda e: e.activation(out=esink[:, :], in_=sk[:, :], func=AF.Exp), reads=["sk"], writes=["esink"])
        fw.op("act", lambda e: e.activation(out=lb3[:, :, :, :], in_=lb3[:, :, :, :], func=AF.Exp), reads=["lb3"], writes=["lb3"])
        fw.op("dve", lambda e: e.tensor_add(out=lbs[:, :, :], in0=lb3[:, :, 0, :], in1=lb3[:, :, 1, :]), reads=["lb3"], writes=["lbs"])
        fw.op("dve", lambda e: e.tensor_add(out=lbs[:, :, :], in0=lbs[:, :, :], in1=lb3[:, :, 2, :]), reads=["lb3", "lbs"], writes=["lbs"])
        fw.op("dve", lambda e: e.reciprocal(out=lbs[:, :, :], in_=lbs[:, :, :]), reads=["lbs"], writes=["lbs"])
        fw.op("dve", lambda e: e.tensor_mul(out=lbt[:, :, :], in0=lb3[:, :, 0, :], in1=lbs[:, :, :]), reads=["lb3", "lbs"], writes=["lbt"])
        fw.op("dve", lambda e: e.tensor_scalar(out=omlb[:, :, :], in0=lbt[:, :, :], scalar1=-1.0, scalar2=1.0,
                                               op0=ALU.mult, op1=ALU.add), reads=["lbt"], writes=["omlb"])
        pi = 0
        for l in range(2):
            for q in range(32):
                w = wt[pi % 2]
                wk = ("adaw", pi % 2)
                src = ada_w[l, :, q * 384:(q + 1) * 384].rearrange("(kc p) j -> p kc j", p=128)
                ld("sp", w[:, :, :], src, writes=[wk])
                for j3 in range(3):
                    c = q * 3 + j3
                    bk = (pi * 3 + j3) % 8
                    pb = bank(bk, 2)
                    mm_group(pb, [(w[:, kc, j3 * 128:(j3 + 1) * 128], scv[:, kc, :]) for kc in range(KC)],
                             [wk, "scv"], [PSK[bk]])
                    fw.op("dve", lambda e, pb=pb, l=l, c=c: e.tensor_scalar(
                        out=modT[:, l, c, :], in0=pb, scalar1=adab[:, l, c:c + 1], scalar2=None, op0=ALU.add),
                        reads=[PSK[bk], "adab"], writes=["modT"])
                pi += 1
        fw.op("dve", lambda e: e.tensor_scalar_add(out=mod1[:, :, :, :], in0=modT[:, :, :, :], scalar1=1.0),
              reads=["modT"], writes=["mod1"])
        for l in range(2):
            for w_, c0 in enumerate((32, 80)):
                fw.op("dve", lambda e, l=l, c0=c0: e.tensor_copy(out=tr16[:, :], in_=modT[:, l, c0:c0 + 16, 0]),
                      reads=["modT"], writes=["tr16"])
                fw.op("pe", lambda e: e.transpose(out=bank(0, 128)[0:16, :], in_=tr16[:, :], identity=ident_f[:, :]),
                      reads=["tr16", "ident_f"], writes=[PSK[0]])
                fw.op("dve", lambda e: e.tensor_copy(out=tr16o[:, :], in_=bank(0, 128)[0:16, :]), reads=[PSK[0]], writes=["tr16o"])
                ld("sp", grow[l][w_].rearrange("o (c p) -> (o c) p", p=128), tr16o[:, :], reads=["tr16o"], writes=[("grow", l, w_)])
        fw.barrier()

    def mcol(l, c, v=0, one=False):
        t = mod1 if one else modT
        return t[:, l, c, v:v + 1]


    def psbf(b):
        return ps[:, b * 512:(b + 1) * 512].bitcast(BF16)

    def bc(ap2, n):
        return ap2.unsqueeze(2).to_broadcast([128, 8, n])

    def hg_pass(d, qeT, keT, dec, vi, nch, S, Sk, Sd, Sbf, ktok, scb, ocb, kkey, vkey):
        order = range(nch) if d == 0 else range(nch - 1, -1, -1)
        PSS = ps[:, 6 * 512:8 * 512]
        psk = [PSK[6], PSK[7]]
        if d == 0 and qeT is not None:
            fw.op("act", lambda e: e.copy(out=Sbf[:, :, :], in_=S[:, :, :]), reads=[Sk], writes=["Sbf"])
        for c in order:
            s0 = 0 if c % 2 == 0 else 3
            cs = slice(c * 64, (c + 1) * 64)

            def tr(e, c=c, s0=s0, cs=cs):
                ins = None
                for h in range(8):
                    ins = e.transpose(out=psbf(s0)[0:64, h * 128:(h + 1) * 128], in_=keT[:, h, cs], identity=ident_b[:, :])
                return ins
            fw.op("pe", tr, reads=[kkey + "ke", "ident_b"], writes=[PSK[s0]])
            fw.op("act", lambda e, s0=s0: e.copy(out=ktok[:, :, :], in_=psbf(s0)[0:64, :].rearrange("p (h k) -> p h k", h=8)),
                  reads=[PSK[s0]], writes=["ktok"])
            if qeT is not None:
                def sc(e, s0=s0, cs=cs):
                    ins = None
                    for h in range(8):
                        ins = e.matmul(bank(s0 + 1)[0:64, h * 64:(h + 1) * 64], lhsT=keT[:, h, cs], rhs=qeT[:, h, cs], start=True, stop=True)
                    return ins
                fw.op("pe", sc, reads=[kkey + "ke", kkey + "qe"], writes=[PSK[s0 + 1]])
                fw.op("dve", lambda e, s0=s0: e.tensor_tensor(out=scb[:, :], in0=bank(s0 + 1)[0:64, :], in1=hmask_b[:, d, :], op=ALU.mult),
                      reads=[PSK[s0 + 1], "hmask_b"], writes=["scb"])
            if d == 1:
                fw.op("dve", lambda e, c=c: e.tensor_tensor(out=Sd[:, :, :], in0=S[:, :, :], in1=bc(dec[:, :, c], 128), op=ALU.mult),
                      reads=[Sk, kkey + "dec"], writes=["Sd"])
                if qeT is not None:
                    fw.op("act", lambda e: e.copy(out=Sbf[:, :, :], in_=Sd[:, :, :]), reads=["Sd"], writes=["Sbf"])
            if qeT is not None:
                def om(e, c=c, s0=s0, cs=cs):
                    ins = None
                    for h in range(8):
                        o = bank(s0 + 2)[:, h * 64:(h + 1) * 64]
                        e.matmul(o, lhsT=Sbf[:, h, :], rhs=qeT[:, h, cs], start=True, stop=False)
                        ins = e.matmul(o, lhsT=vi[:, c, h * 128:(h + 1) * 128], rhs=scb[:, h * 64:(h + 1) * 64], start=False, stop=True)
                    return ins
                fw.op("pe", om, reads=["Sbf", kkey + "qe", vkey, "scb"], writes=[PSK[s0 + 2]])
                ocb(c, bank(s0 + 2), PSK[s0 + 2])
            Ssrc = S if d == 0 else Sd

            def stm(e, c=c, Ssrc=Ssrc):
                ins = None
                for h in range(8):
                    o = PSS[:, h * 128:(h + 1) * 128]
                    e.matmul(o, lhsT=ident_f[:, :], rhs=Ssrc[:, h, :], start=True, stop=False)
                    ins = e.matmul(o, lhsT=ktok[:, h, :], rhs=vi[:, c, h * 128:(h + 1) * 128], start=False, stop=True)
                return ins
            fw.op("pe", stm, reads=[Sk, "Sd", "ktok", vkey, "ident_f"], writes=psk)
            for hf in range(2):
                src = PSS[:, hf * 512:(hf + 1) * 512].rearrange("p (h v) -> p h v", h=4)
                dst = S[:, hf * 4:(hf + 1) * 4, :]
                if d == 0:
                    fw.op("dve", lambda e, src=src, dst=dst, c=c, hf=hf: e.tensor_tensor(
                        out=dst, in0=src, in1=dec[:, hf * 4:(hf + 1) * 4, c].unsqueeze(2).to_broadcast([128, 4, 128]), op=ALU.mult),
                        reads=[psk[hf], kkey + "dec"], writes=[Sk])
                else:
                    fw.op("act", lambda e, src=src, dst=dst: e.copy(out=dst, in_=src), reads=[psk[hf]], writes=[Sk])
            if d == 0 and qeT is not None:
                fw.op("act", lambda e: e.copy(out=Sbf[:, :, :], in_=S[:, :, :]), reads=[Sk], writes=["Sbf"])

    tcnt = {"n": 0}

    def trans_mod(xt, xk, dstT, col0, scale_fn, bias_fn, wkey, tbanks=(0, 1), f32copy=None, npart=128):
        for q in range(4):
            bk = tbanks[tcnt["n"] % 2]
            tcnt["n"] += 1

            def tr(e, q=q, bk=bk):
                ins = None
                for j in range(4):
                    kc = q * 4 + j
                    ins = e.transpose(out=bank(bk, npart, j * 128), in_=xt[0:npart, kc * 128:(kc + 1) * 128], identity=ident_f[0:npart, 0:npart])
                return ins
            fw.op("pe", tr, reads=[xk, "ident_f"], writes=[PSK[bk]])
            for j in range(4):
                kc = q * 4 + j
                src = bank(bk, npart, j * 128)
                dst = dstT[:, kc, col0:col0 + npart]
                if (q % 2) == 0:
                    fw.op("act", lambda e, src=src, dst=dst, kc=kc: e.activation(
                        out=dst, in_=src, func=AF.Identity, scale=scale_fn(kc), bias=bias_fn(kc)),
                        reads=[PSK[bk], "modT", "mod1"], writes=[wkey])
                else:
                    fw.op("dve", lambda e, src=src, dst=dst, kc=kc: e.tensor_scalar(
                        out=dst, in0=src, scalar1=scale_fn(kc), scalar2=bias_fn(kc), op0=ALU.mult, op1=ALU.add),
                        reads=[PSK[bk], "modT", "mod1"], writes=[wkey])
                if f32copy is not None:
                    f32copy(kc, src, PSK[bk], "act" if (q % 2) == 0 else "dve")

    with contextlib.ExitStack() as p1:
        hxT = sb("hxT", [128, KC, TE], BF16, stack=p1)
        hcT = sb("hcT", [128, KC, NCTX], BF16, stack=p1)
        wbuf = [sb("wbuf%d" % i, [128, KC, 256], BF16, stack=p1) for i in range(3)]
        wcnt = {"n": 0}
        pcnt = {"n": 0}
        PB = (2, 3, 4, 5)

        def load_wpiece(col0, ncols=256):
            i = wcnt["n"] % 3
            wcnt["n"] += 1
            src = win_in[:, col0:col0 + ncols].rearrange("(kc p) j -> p kc j", p=128)
            ld("pool", wbuf[i][:, :, 0:ncols], src, writes=[("wbuf", i)])
            return wbuf[i], ("wbuf", i)

        def proj_fm(wb, wk, cb, srcT, skeys, tok0, ntok, evac):
            bk = PB[pcnt["n"] % 4]
            pcnt["n"] += 1
            mm_group(bank(bk, ntok), [(wb[:, kc, cb * 128:(cb + 1) * 128], srcT[:, kc, tok0:tok0 + ntok]) for kc in range(KC)],
                     [wk] + list(skeys), [PSK[bk]])
            evac(bank(bk, ntok), PSK[bk])

        def proj_tm(wb, wk, ncols, srcT, skeys, tok0, nt, evac):
            bk = PB[pcnt["n"] % 4]
            pcnt["n"] += 1
            mm_group(bank(bk, ncols)[0:nt, :], [(srcT[:, kc, tok0:tok0 + nt], wb[:, kc, 0:ncols]) for kc in range(KC)],
                     [wk] + list(skeys), [PSK[bk]])
            evac(bank(bk, ncols)[0:nt, :], PSK[bk])

        for seg in range(NSEG):
            pxt = contextlib.ExitStack()
            xts = [sb("xt%d_%d" % (i, seg), [128, D], stack=pxt) for i in range(2)]
            for tt in range(10):
                g0 = seg * 1024 - 128 + tt * 128
                if g0 < 0 or g0 >= SEQL:
                    fw.op("pool", lambda e, tt=tt: e.memset(hxT[:, :, tt * 128:(tt + 1) * 128], 0.0), writes=[("hxT", tt)])
                    continue
                xt = xts[tt % 2]
                xk = ("xt", tt % 2)
                ld("sp", xt[:, :], x_in[g0:g0 + 128, :], writes=[xk])
                trans_mod(xt, xk, hxT, tt * 128, lambda kc: mcol(0, 16 + kc, 0, True), lambda kc: mcol(0, kc, 0), ("hxT", tt))
            if seg == 0:
                for tt in range(2):
                    xt = xts[tt % 2]
                    xk = ("xt", tt % 2)
                    ld("sp", xt[:, :], ctx_in[tt * 128:(tt + 1) * 128, :], writes=[xk])
                    trans_mod(xt, xk, hcT, tt * 128, lambda kc: mcol(0, 16 + kc, 1, True), lambda kc: mcol(0, kc, 1), ("hcT", tt))
            fw.barrier()
            pxt.close()
            HXK = [("hxT", tt) for tt in range(10)]
            HCK = [("hcT", 0), ("hcT", 1)]

            def hx_keys(tok0, ntok):
                return [("hxT", t) for t in range(tok0 // 128, (tok0 + ntok + 127) // 128)]

            with contextlib.ExitStack() as pa:
                qT = sb("qT", [128, 8, T], BF16, stack=pa)
                kT = sb("kT", [128, 4, TE], BF16, stack=pa)
                v_tok = sb("v_tok", [128, 10, 512], BF16, stack=pa)
                attT = sb("attT", [128, 8, T], BF16, stack=pa)
                rc = sb("rc", [128, TE], stack=pa)
                rs = sb("rs", [128, TE], stack=pa)
                qraw = [sb("qraw%d" % i, [128, 512], BF16, stack=pa) for i in range(2)]
                rt1 = [sb("rt1_%d" % i, [128, 512], stack=pa) for i in range(2)]
                rt2 = [sb("rt2_%d" % i, [128, 512], stack=pa) for i in range(2)]
                pT = [sb("pT%d" % i, [128, 5 * 256], BF16, stack=pa) for i in range(2)]
                den = [sb("den%d" % i, [128, 256], stack=pa) for i in range(2)]
                ld("sp", rc[:, :], cos_in[:, seg * 1024:seg * 1024 + TE], writes=["rc"])
                ld("sp", rs[:, :], sin_in[:, seg * 1024:seg * 1024 + TE], writes=["rs"])
                rcnt = {"n": 0}

                def rope_evac(dst, e0, ntok):
                    def evac(pb, pk):
                        i = rcnt["n"] % 2
                        rcnt["n"] += 1
                        rb = 6 + i
                        qr, t1, t2 = qraw[i], rt1[i], rt2[i]
                        fw.op("act", lambda e: e.copy(out=qr[:, 0:ntok], in_=pb), reads=[pk], writes=[("qraw", i)])
                        fw.op("pe", lambda e: e.matmul(bank(rb, ntok), lhsT=ropeR_b[:, :], rhs=qr[:, 0:ntok], start=True, stop=True),
                              reads=[("qraw", i), "ropeR_b"], writes=[PSK[rb]])
                        fw.op("dve", lambda e: e.tensor_mul(out=t1[:, 0:ntok], in0=qr[:, 0:ntok], in1=rc[:, e0:e0 + ntok]),
                              reads=[("qraw", i), "rc"], writes=[("rt1", i)])
                        fw.op("dve", lambda e: e.tensor_mul(out=t2[:, 0:ntok], in0=bank(rb, ntok), in1=rs[:, e0:e0 + ntok]),
                              reads=[PSK[rb], "rs"], writes=[("rt2", i)])
                        fw.op("dve", lambda e: e.tensor_add(out=dst, in0=t1[:, 0:ntok], in1=t2[:, 0:ntok]),
                              reads=[("rt1", i), ("rt2", i)], writes=["qk"])
                    return evac

                for pc in range(4):
                    wb, wk = load_wpiece(pc * 256)
                    for hb in range(2):
                        for th in range(2):
                            proj_fm(wb, wk, hb, hxT, hx_keys(128 + th * 512, 512), 128 + th * 512, 512,
                                    rope_evac(qT[:, pc * 2 + hb, th * 512:(th + 1) * 512], 128 + th * 512, 512))
                for pc in range(2):
                    wb, wk = load_wpiece(1024 + pc * 256)
                    for hb in range(2):
                        for (t0, nt) in ((0, 512), (512, 512), (1024, 256)):
                            proj_fm(wb, wk, hb, hxT, hx_keys(t0, nt), t0, nt, rope_evac(kT[:, pc * 2 + hb, t0:t0 + nt], t0, nt))
                        if seg == 0:
                            h = pc * 2 + hb
                            proj_fm(wb, wk, hb, hcT, HCK, 0, NCTX,
                                    lambda pb, pk, h=h: fw.op("act", lambda e: e.copy(out=kcT[:, h, :], in_=pb), reads=[pk], writes=["kcT"]))
                for pc in range(2):
                    wb, wk = load_wpiece(1536 + pc * 256)
                    for tt in range(10):
                        proj_tm(wb, wk, 256, hxT, [("hxT", tt)], tt * 128, 128,
                                lambda pb, pk, tt=tt, pc=pc: fw.op("act" if tt % 2 else "dve", (lambda e: e.copy(out=v_tok[:, tt, pc * 256:(pc + 1) * 256], in_=pb)) if tt % 2 else
                                                                   (lambda e: e.tensor_copy(out=v_tok[:, tt, pc * 256:(pc + 1) * 256], in_=pb)), reads=[pk], writes=["v_tok"]))
                    if seg == 0:
                        for tt in range(2):
                            proj_tm(wb, wk, 256, hcT, HCK, tt * 128, 128,
                                    lambda pb, pk, tt=tt, pc=pc: fw.op("act", lambda e: e.copy(out=vc_tok[:, tt, pc * 256:(pc + 1) * 256], in_=pb), reads=[pk], writes=["vc_tok"]))
                ai = 0
                for h in range(4):
                    for n in range(8):
                        blocks = []
                        if not (seg == 0 and n == 0):
                            blocks.append(("l", n, 0))
                        blocks.append(("l", n + 1, None))
                        if not (seg == NSEG - 1 and n == 7):
                            blocks.append(("l", n + 2, 1))
                        blocks.append(("c", 0, None))
                        blocks.append(("c", 1, None))
                        nb = len(blocks)
                        st = ai % 2
                        ai += 1
                        sb0 = 0 if st == 0 else 4
                        sk_ = [PSK[sb0], PSK[sb0 + 1], PSK[sb0 + 2]]
                        ok_ = PSK[sb0 + 3]
                        rhs_q = qT[:, 2 * h:2 * h + 2, n * 128:(n + 1) * 128]

                        def scores(e, blocks=blocks, sb0=sb0, h=h, rhs_q=rhs_q):
                            ins = None
                            for bi, (kind, idx, _) in enumerate(blocks):
                                lhs = kT[:, h, idx * 128:(idx + 1) * 128] if kind == "l" else kcT[:, h, idx * 128:(idx + 1) * 128]
                                o = ps[:, sb0 * 512 + bi * 256: sb0 * 512 + (bi + 1) * 256]
                                ins = e.matmul(o, lhsT=lhs, rhs=rhs_q, start=True, stop=True)
                            return ins
                        fw.op("pe", scores, reads=["qk", "kcT"], writes=sk_)
                        p = pT[st]
                        pk_ = ("pT", st)
                        fw.op("act", lambda e, p=p, sb0=sb0, nb=nb: e.activation(
                            out=p[:, 0:nb * 256], in_=ps[:, sb0 * 512: sb0 * 512 + nb * 256], func=AF.Exp, scale=ATT_SCALE),
                            reads=sk_, writes=[pk_])
                        for bi, (kind, idx, mk) in enumerate(blocks):
                            if mk is not None:
                                pv = p[:, bi * 256:(bi + 1) * 256].rearrange("p (g t) -> p g t", g=2)
                                fw.op("dve", lambda e, pv=pv, mk=mk: e.tensor_tensor(
                                    out=pv, in0=pv, in1=amask_b[:, mk, :].unsqueeze(1).to_broadcast([128, 2, 128]), op=ALU.mult),
                                    reads=[pk_, "amask_b"], writes=[pk_])

                        def pv_mm(e, blocks=blocks, sb0=sb0, h=h, p=p, nb=nb):
                            ins = None
                            oo = ps[:, (sb0 + 3) * 512:(sb0 + 3) * 512 + 256]
                            os_ = ps[:, (sb0 + 3) * 512 + 256:(sb0 + 3) * 512 + 512]
                            for bi, (kind, idx, _) in enumerate(blocks):
                                vv = v_tok[:, idx, h * 128:(h + 1) * 128] if kind == "l" else vc_tok[:, idx, h * 128:(h + 1) * 128]
                                ins = e.matmul(oo, lhsT=vv, rhs=p[:, bi * 256:(bi + 1) * 256], start=(bi == 0), stop=(bi == nb - 1))
                            for bi in range(nb):
                                ins = e.matmul(os_, lhsT=ones_b[:, :], rhs=p[:, bi * 256:(bi + 1) * 256], start=(bi == 0), stop=(bi == nb - 1))
                            return ins
                        fw.op("pe", pv_mm, reads=[pk_, "v_tok", "vc_tok", "ones_b"], writes=[ok_])
                        dn = den[st]
                        dk = ("den", st)
                        for g in range(2):
                            fw.op("dve", lambda e, g=g, dn=dn, sb0=sb0, h=h: e.tensor_scalar(
                                out=dn[:, g * 128:(g + 1) * 128], in0=ps[:, (sb0 + 3) * 512 + 256 + g * 128:(sb0 + 3) * 512 + 256 + (g + 1) * 128],
                                scalar1=esink[:, 2 * h + g:2 * h + g + 1], scalar2=None, op0=ALU.add), reads=[ok_, "esink"], writes=[dk])
                        fw.op("dve", lambda e, dn=dn: e.reciprocal(out=dn[:, :], in_=dn[:, :]), reads=[dk], writes=[dk])
                        fw.op("dve", lambda e, dn=dn, sb0=sb0, h=h, n=n: e.tensor_tensor(
                            out=attT[:, 2 * h:2 * h + 2, n * 128:(n + 1) * 128],
                            in0=ps[:, (sb0 + 3) * 512:(sb0 + 3) * 512 + 256].rearrange("p (g t) -> p g t", g=2),
                            in1=dn[:, :].rearrange("p (g t) -> p g t", g=2), op=ALU.mult), reads=[ok_, dk], writes=["attT"])
                ld("sp", att_d[seg].rearrange("p (h t) -> p h t", h=8), attT[:, :, :], reads=["attT"], writes=[("att_d", seg)])
                if upto == 1 and seg == 0:
                    ld("pool", dbg.rearrange("p (h t) -> p h t", h=8), attT[:, :, :], reads=["attT"])
                fw.barrier()
            if upto == 1:
                continue

            with contextlib.ExitStack() as ph:
                qhT = sb("qhT", [128, 8, T], BF16, stack=ph)
                tf = sb("tf", [128, T], stack=ph)
                tk = sb("tk", [128, T], stack=ph)
                tg = sb("tg", [128, T], stack=ph)
                tP = sb("tP", [128, T], stack=ph)
                te1 = sb("te1", [128, T], stack=ph)
                te2 = sb("te2", [128, T], stack=ph)
                qe_s = [sb("qe_s%d" % i, [128, T], BF16, stack=ph) for i in range(2)]
                ke_s = [sb("ke_s%d" % i, [128, T], BF16, stack=ph) for i in range(2)]
                decst = sb("decst", [128, 8, 16], stack=ph)
                vi64 = sb("vi64", [64, 16, 1024], BF16, stack=ph)
                scnt = {"n": 0}
                for pc in range(4):
                    wb, wk = load_wpiece(2048 + pc * 256)
                    for hb in range(2):
                        for th in range(2):
                            dst = qhT[:, pc * 2 + hb, th * 512:(th + 1) * 512]
                            proj_fm(wb, wk, hb, hxT, hx_keys(128 + th * 512, 512), 128 + th * 512, 512,
                                    lambda pb, pk, dst=dst: fw.op("act", lambda e: e.activation(out=dst, in_=pb, func=AF.Silu), reads=[pk], writes=["qhT"]))

                def prep(d, h, wb, wk, hb, srcT, skeys_fn, tok0, ntok, qsrc, qe_out, ke_out, dec_out, okey):
                    nch = ntok // 64
                    for t0 in range(0, ntok, 512):
                        nt = min(512, ntok - t0)
                        proj_fm(wb, wk, hb, srcT, skeys_fn(tok0 + t0, nt), tok0 + t0, nt,
                                lambda pb, pk, t0=t0, nt=nt: fw.op("act", lambda e: e.activation(out=tf[:, t0:t0 + nt], in_=pb, func=AF.Sigmoid), reads=[pk], writes=["tf"]))
                    fw.op("dve", lambda e: e.tensor_scalar(out=tf[:, 0:ntok], in0=tf[:, 0:ntok], scalar1=omlb[:, d, h:h + 1], scalar2=lbt[:, d, h:h + 1],
                                                           op0=ALU.mult, op1=ALU.add), reads=["tf", "omlb", "lbt"], writes=["tf"])
                    fw.op("act", lambda e: e.activation(out=tg[:, 0:ntok], in_=tf[:, 0:ntok], func=AF.Ln), reads=["tf"], writes=["tg"])
                    fw.op("pool", lambda e: e.tensor_scalar(out=tk[:, 0:ntok], in0=tf[:, 0:ntok], scalar1=-1.0, scalar2=1.0, op0=ALU.mult, op1=ALU.add),
                          reads=["tf"], writes=["tk"])
                    fw.op("dve", lambda e: e.tensor_tensor_scan(out=tP[:, 0:ntok], data0=resetp[:, 0:ntok], data1=tg[:, 0:ntok], initial=0.0,
                                                                op0=ALU.mult, op1=ALU.add), reads=["tg", "resetp"], writes=["tP"])
                    pend = tP[:, 0:ntok].rearrange("p (c j) -> p c j", j=64)[:, :, 63]
                    if d == 0:
                        fw.op("act", lambda e: e.activation(out=te1[:, 0:ntok], in_=tP[:, 0:ntok], func=AF.Exp), reads=["tP"], writes=["te1"])
                        fw.op("act", lambda e: e.activation(out=te2[:, 0:ntok], in_=tP[:, 0:ntok], func=AF.Exp, scale=-1.0), reads=["tP"], writes=["te2"])
                    else:
                        fw.op("pool", lambda e: e.tensor_sub(out=tg[:, 0:ntok], in0=tP[:, 0:ntok], in1=tg[:, 0:ntok]), reads=["tP", "tg"], writes=["tg"])
                        fw.op("act", lambda e: e.activation(out=te1[:, 0:ntok], in_=tg[:, 0:ntok], func=AF.Exp, scale=-1.0), reads=["tg"], writes=["te1"])
                        fw.op("act", lambda e: e.activation(out=te2[:, 0:ntok], in_=tg[:, 0:ntok], func=AF.Exp), reads=["tg"], writes=["te2"])
                    fw.op("act", lambda e: e.activation(out=dec_out[:, h, 0:nch], in_=pend, func=AF.Exp), reads=["tP"], writes=[okey + "dec"])
                    if qsrc is not None:
                        fw.op("dve", lambda e: e.tensor_mul(out=qe_out, in0=qsrc, in1=te1[:, 0:ntok]), reads=["qhT", "te1"], writes=[okey + "qe"])
                    fw.op("pool", lambda e: e.tensor_mul(out=ke_out, in0=tk[:, 0:ntok], in1=te2[:, 0:ntok]), reads=["tk", "te2"], writes=[okey + "ke"])

                if seg == 0:
                    kce = [sb("kce%d" % d, [128, 8, NCTX], BF16, stack=ph) for d in range(2)]
                    decc = [sb("decc%d" % d, [128, 8, 4], stack=ph) for d in range(2)]
                    vic = sb("vic", [64, 4, 1024], BF16, stack=ph)
                for d in range(2):
                    for pc in range(4):
                        wb, wk = load_wpiece(3072 + d * 1024 + pc * 256)
                        for hb in range(2):
                            h = pc * 2 + hb
                            i = scnt["n"] % 2
                            scnt["n"] += 1
                            prep(d, h, wb, wk, hb, hxT, hx_keys, 128, T, qhT[:, h, :], qe_s[i][:, :], ke_s[i][:, :], decst, "st%d" % i)
                            ld("sp", qe_d[d][seg][:, h * 1024:(h + 1) * 1024], qe_s[i][:, :], reads=["st%dqe" % i], writes=[("qe_d", d, seg)])
                            ld("sp", ke_d[d][seg][:, h * 1024:(h + 1) * 1024], ke_s[i][:, :], reads=["st%dke" % i], writes=[("ke_d", d, seg)])
                            if seg == 0:
                                prep(d, h, wb, wk, hb, hcT, lambda a, b: HCK, 0, NCTX, None, None, kce[d][:, h, :], decc[d], "cx%d" % d)
                    ld("sp", dec_d[d][seg].rearrange("p (h c) -> p h c", h=8), decst[:, :, :], reads=["st0dec", "st1dec"], writes=[("dec_d", d, seg)])
                for pc in range(4):
                    wb, wk = load_wpiece(5120 + pc * 256)
                    for c in range(16):
                        proj_tm(wb, wk, 256, hxT, hx_keys(128 + c * 64, 64), 128 + c * 64, 64,
                                lambda pb, pk, c=c, pc=pc: fw.op("act" if c % 2 else "dve", (lambda e: e.copy(out=vi64[:, c, pc * 256:(pc + 1) * 256], in_=pb)) if c % 2 else
                                                                 (lambda e: e.tensor_copy(out=vi64[:, c, pc * 256:(pc + 1) * 256], in_=pb)), reads=[pk], writes=["vi64"]))
                    if seg == 0:
                        for c in range(4):
                            proj_tm(wb, wk, 256, hcT, HCK, c * 64, 64,
                                    lambda pb, pk, c=c, pc=pc: fw.op("act", lambda e: e.copy(out=vic[:, c, pc * 256:(pc + 1) * 256], in_=pb), reads=[pk], writes=["vic"]))
                ld("sp", vi_d[seg].rearrange("p (c v) -> p c v", c=16), vi64[:, :, :], reads=["vi64"], writes=[("vi_d", seg)])
                for pc in range(4):
                    wb, wk = load_wpiece(6144 + pc * 256)
                    for hb in range(2):
                        h = pc * 2 + hb
                        i = scnt["n"] % 2
                        scnt["n"] += 1
                        for th in range(2):
                            proj_fm(wb, wk, hb, hxT, hx_keys(128 + th * 512, 512), 128 + th * 512, 512,
                                    lambda pb, pk, th=th: fw.op("act", lambda e: e.activation(out=tf[:, th * 512:(th + 1) * 512], in_=pb, func=AF.Silu), reads=[pk], writes=["tf"]))
                        fw.op("dve", lambda e, i=i, h=h: e.tensor_scalar(out=qe_s[i][:, :], in0=tf[:, :], scalar1=normg[:, h:h + 1], scalar2=None, op0=ALU.mult),
                              reads=["tf", "normg"], writes=["st%dqe" % i])
                        ld("sp", g_d[seg][:, h * 1024:(h + 1) * 1024], qe_s[i][:, :], reads=["st%dqe" % i], writes=[("g_d", seg)])
                if seg == 0 and not cfg.get('skip_cx'):
                    with contextlib.ExitStack() as pcx:
                        Sbf_c = sb("Sbf_c", [128, 8, 128], BF16, stack=pcx)
                        Sd_c = sb("Sd_c", [128, 8, 128], stack=pcx)
                        ktok_c = sb("ktok_c", [64, 8, 128], BF16, stack=pcx)
                        for d in range(2):
                            fw.op("dve", lambda e, d=d: e.memset(S0[d][:, :, :], 0.0), writes=[("S0", d)])
                            hg_pass(d, None, kce[d], decc[d], vic, 4, S0[d], ("S0", d), Sd_c, Sbf_c, ktok_c, None, None, "cx%d" % d, "vic")
                fw.barrier()
        fw.barrier()

    if upto >= 2 and not cfg.get('skip_s2'):
      with contextlib.ExitStack() as p2:
        qeT = sb("qeT", [128, 8, T], BF16, stack=p2)
        keT = sb("keT", [128, 8, T], BF16, stack=p2)
        vi2 = sb("vi2", [64, 16, 1024], BF16, stack=p2)
        dect = sb("dect", [128, 8, 16], stack=p2)
        oF = sb("oF", [128, 8, T], BF16, stack=p2)
        gTn = sb("gTn", [128, 8, T], BF16, stack=p2)
        Sst = sb("Sst", [128, 8, 128], stack=p2)
        Sd2 = sb("Sd2", [128, 8, 128], stack=p2)
        Sbf2 = sb("Sbf2", [128, 8, 128], BF16, stack=p2)
        ktok2 = sb("ktok2", [64, 8, 128], BF16, stack=p2)
        scb2 = sb("scb2", [64, 512], BF16, stack=p2)
        osum = sb("osum", [128, 512], stack=p2)
        sq = sb("sq", [128, 512], BF16, stack=p2)
        rstd = sb("rstd", [128, 512], stack=p2)
        for d in range(2):
            fw.op("dve", lambda e, d=d: e.tensor_copy(out=Sst[:, :, :], in_=S0[d][:, :, :]), reads=[("S0", d)], writes=["Sst"])
            for seg in (range(NSEG) if d == 0 else range(NSEG - 1, -1, -1)):
                ld("sp", qeT[:, :, :], qe_d[d][seg].rearrange("p (h t) -> p h t", h=8), reads=[("qe_d", d, seg)], writes=["s2qe"])
                ld("sp", keT[:, :, :], ke_d[d][seg].rearrange("p (h t) -> p h t", h=8), reads=[("ke_d", d, seg)], writes=["s2ke"])
                ld("sp", dect[:, :, :], dec_d[d][seg].rearrange("p (h c) -> p h c", h=8), reads=[("dec_d", d, seg)], writes=["s2dec"])
                ld("sp", vi2[:, :, :], vi_d[seg].rearrange("p (c v) -> p c v", c=16), reads=[("vi_d", seg)], writes=["vi2"])
                if d == 1:
                    ld("sp", oF[:, :, :], of_d[seg].rearrange("p (h t) -> p h t", h=8), reads=[("of_d", seg)], writes=["oF"])
                    ld("sp", gTn[:, :, :], g_d[seg].rearrange("p (h t) -> p h t", h=8), reads=[("g_d", seg)], writes=["gTn"])

                def ocb(c, ob, ok, d=d):
                    cs = slice(c * 64, (c + 1) * 64)
                    obv = ob.rearrange("p (h t) -> p h t", h=8)
                    if d == 0:
                        fw.op("act", lambda e: e.copy(out=oF[:, :, cs], in_=obv), reads=[ok], writes=["oF"])
                        return
                    msb = (0 if c % 2 == 0 else 3) + 1
                    osv = osum[:, :].rearrange("p (h t) -> p h t", h=8)
                    fw.op("dve", lambda e: e.tensor_tensor(out=osv, in0=obv, in1=oF[:, :, cs], op=ALU.add), reads=[ok, "oF"], writes=["osum"])
                    fw.op("act", lambda e: e.activation(out=sq[:, :], in_=osum[:, :], func=AF.Square), reads=["osum"], writes=["sq"])
                    fw.op("pe", lambda e: e.matmul(bank(msb), lhsT=ones_b[:, :], rhs=sq[:, :], start=True, stop=True),
                          reads=["sq", "ones_b"], writes=[PSK[msb]])
                    fw.op("dve", lambda e: e.tensor_scalar(out=rstd[:, :], in0=bank(msb), scalar1=1.0 / 128.0, scalar2=NORM_EPS, op0=ALU.mult, op1=ALU.add),
                          reads=[PSK[msb]], writes=["rstd"])
                    fw.op("act", lambda e: e.activation(out=rstd[:, :], in_=rstd[:, :], func=AF.Sqrt), reads=["rstd"], writes=["rstd"])
                    fw.op("dve", lambda e: e.reciprocal(out=rstd[:, :], in_=rstd[:, :]), reads=["rstd"], writes=["rstd"])
                    fw.op("dve", lambda e: e.tensor_mul(out=osum[:, :], in0=osum[:, :], in1=rstd[:, :]), reads=["osum", "rstd"], writes=["osum"])
                    fw.op("dve", lambda e: e.tensor_tensor(out=oF[:, :, cs], in0=osv, in1=gTn[:, :, cs], op=ALU.mult),
                          reads=["osum", "gTn", "oF"], writes=["oF"])
                hg_pass(d, qeT, keT, dect, vi2, 16, Sst, "Sst", Sd2, Sbf2, ktok2, scb2, ocb, "s2", "vi2")
                dst = of_d[seg] if d == 0 else hg_d[seg]
                ld("sp", dst.rearrange("p (h t) -> p h t", h=8), oF[:, :, :], reads=["oF"],
                   writes=[("of_d", seg) if d == 0 else ("hg_d", seg)])
                if upto == 2 and d == 1 and seg == 0:
                    ld("pool", dbg.rearrange("p (h t) -> p h t", h=8), oF[:, :, :], reads=["oF"])
        fw.barrier()

    rtw = sb("rtw", [128, 2, KC, NR])
    rtb = sb("rtb", [128, 2, NR])
    for l in range(2):
        ld("sp", rtw[:, l, :, :], rtw_in[l].rearrange("(kc p) r -> p kc r", p=128), writes=["rtw"])
        ld("sp", rtb[:, l, :], rtb_in[l:l + 1, :].partition_broadcast(128), writes=["rtb"])

    def bcast_rows(dst, src_row, key):
        ld("sp", dst[:, :], src_row.partition_broadcast(128), writes=[key])

    def layer_norm(zt, zk, xn, xnk, lngb, lnbb, st, keys):
        stats, mv, rs_, nmr = st
        if cfg.get("no_ln"):
            fw.op("dve", lambda e: e.tensor_copy(out=xn[:, :], in_=zt[:, :]), reads=[zk], writes=[xnk])
            return
        for q in range(4):
            fw.op("dve", lambda e, q=q: e.bn_stats(out=stats[:, q, :], in_=zt[:, q * 512:(q + 1) * 512]), reads=[zk], writes=["lnst"])
        fw.op("dve", lambda e: e.bn_aggr(out=mv[:, :], in_=stats[:, :, :].rearrange("p a b -> p (a b)")), reads=["lnst"], writes=["lnmv"])
        fw.op("dve", lambda e: e.tensor_scalar_add(out=rs_[:, :], in0=mv[:, 1:2], scalar1=LN_EPS), reads=["lnmv"], writes=["lnrs"])
        fw.op("act", lambda e: e.activation(out=rs_[:, :], in_=rs_[:, :], func=AF.Sqrt), reads=["lnrs"], writes=["lnrs"])
        fw.op("dve", lambda e: e.reciprocal(out=rs_[:, :], in_=rs_[:, :]), reads=["lnrs"], writes=["lnrs"])
        fw.op("dve", lambda e: e.tensor_mul(out=nmr[:, :], in0=mv[:, 0:1], in1=rs_[:, 0:1]), reads=["lnmv", "lnrs"], writes=["lnnm"])
        fw.op("dve", lambda e: e.tensor_scalar(out=nmr[:, :], in0=nmr[:, :], scalar1=-1.0, scalar2=None, op0=ALU.mult), reads=["lnnm"], writes=["lnnm"])
        fw.op("act", lambda e: e.activation(out=xn[:, :], in_=zt[:, :], func=AF.Identity, scale=rs_[:, 0:1], bias=nmr[:, 0:1]),
              reads=[zk, "lnrs", "lnnm"], writes=[xnk])
        fw.op("pool", lambda e: e.tensor_mul(out=xn[:, :], in0=xn[:, :], in1=lngb[:, :]), reads=[xnk] + keys, writes=[xnk])
        fw.op("pool", lambda e: e.tensor_add(out=xn[:, :], in0=xn[:, :], in1=lnbb[:, :]), reads=[xnk] + keys, writes=[xnk])


    ys_d = [dint("ys_d%d" % s_, [128, KC * 1024], BF16) for s_ in range(NSEG)]

    def pool_mixer(seg):
        nm = "_p%d" % seg
        with contextlib.ExitStack() as pp:
            hx1 = sb("hx1" + nm, [128, KC, 1040], BF16, stack=pp)
            mixed = sb("mixed" + nm, [128, KC, T], BF16, stack=pp)
            ys = sb("ys" + nm, [128, KC, T], BF16, stack=pp)
            uT = sb("uT" + nm, [128, 1040], stack=pp)
            bA = sb("bA" + nm, [128, 1040], stack=pp)
            bB = sb("bB" + nm, [128, 1040], stack=pp)
            pinvb = sb("pinvb" + nm, [128, 4, T], stack=pp)
            xt = sb("xtp" + nm, [128, D], stack=pp)
            xh = sb("xh" + nm, [8, D], stack=pp)
            wpb = [sb("wpb%d" % i + nm, [128, KC, 256], BF16, stack=pp) for i in range(2)]
            wgb = [sb("wgb%d" % i + nm, [128, 4, 512], BF16, stack=pp) for i in range(2)]
            for g in range(4):
                ld("sp", pinvb[:, g, :], pinv_in[0:1, g * SEQL + seg * 1024: g * SEQL + (seg + 1) * 1024].partition_broadcast(128), writes=["pinvb"])
            sc_ = lambda kc: mcol(1, 16 + kc, 0, True)
            sh_ = lambda kc: mcol(1, kc, 0)
            for side, (r0, c0, ok) in enumerate(((seg * 1024 - 8, 0, seg > 0), ((seg + 1) * 1024, 1032, seg < NSEG - 1))):
                if ok:
                    ld("sp", xh[:, :], xl0_d[r0:r0 + 8, :], reads=[("xsrc", 1)], writes=["xh"])
                    trans_mod(xh, "xh", hx1, c0, sc_, sh_, "hx1", npart=8)
                else:
                    fw.op("pool", lambda e, c0=c0: e.memset(hx1[:, :, c0:c0 + 8], 0.0), writes=["hx1"])
            for tile in range(8):
                r0 = seg * 1024 + tile * 128
                ld("sp", xt[:, :], xl0_d[r0:r0 + 128, :], reads=[("xsrc", 1)], writes=["xtp"])
                trans_mod(xt, "xtp", hx1, 8 + tile * 128, sc_, sh_, "hx1")
            fw.barrier()
            pk2 = 0
            for pc in range(8):
                wb = wpb[pc % 2]
                wk = ("wpb", pc % 2)
                ld("pool", wb[:, :, :], pwin_in[:, pc * 256:(pc + 1) * 256].rearrange("(kc p) j -> p kc j", p=128), writes=[wk])
                for cb in range(2):
                    fcn = pc * 2 + cb
                    g = fcn // 4
                    for (t0, nt) in ((0, 8), (8, 512), (520, 512), (1032, 8)):
                        bk = 2 + (pk2 % 4)
                        pk2 += 1
                        mm_group(bank(bk, nt), [(wb[:, kc, cb * 128:(cb + 1) * 128], hx1[:, kc, t0:t0 + nt]) for kc in range(KC)], [wk, "hx1"], [PSK[bk]])
                        fw.op("act", lambda e, bk=bk, t0=t0, nt=nt: e.copy(out=uT[:, t0:t0 + nt], in_=bank(bk, nt)), reads=[PSK[bk]], writes=["uT"])
                    fw.op("dve", lambda e: e.tensor_add(out=bA[:, 1:1040], in0=uT[:, 0:1039], in1=uT[:, 1:1040]), reads=["uT"], writes=["bA"])
                    fin, fk = bA, "bA"
                    if g >= 1:
                        fw.op("dve", lambda e: e.tensor_add(out=bB[:, 2:1039], in0=bA[:, 1:1038], in1=bA[:, 3:1040]), reads=["bA"], writes=["bB"])
                        fin, fk = bB, "bB"
                    if g >= 2:
                        fw.op("dve", lambda e: e.tensor_add(out=bA[:, 4:1036], in0=bB[:, 2:1034], in1=bB[:, 6:1038]), reads=["bB"], writes=["bA"])
                        fin, fk = bA, "bA"
                    if g >= 3:
                        fw.op("dve", lambda e: e.tensor_add(out=bB[:, 8:1032], in0=bA[:, 4:1028], in1=bA[:, 12:1036]), reads=["bA"], writes=["bB"])
                        fin, fk = bB, "bB"
                    fw.op("pool", lambda e, fin=fin, g=g: e.tensor_mul(out=fin[:, 8:1032], in0=fin[:, 8:1032], in1=pinvb[:, g, :]), reads=[fk, "pinvb"], writes=[fk])
                    fw.op("pool", lambda e, fin=fin, fcn=fcn: e.tensor_sub(out=mixed[:, fcn, :], in0=fin[:, 8:1032], in1=uT[:, 8:1032]), reads=[fk, "uT"], writes=["mixed"])
            for g in range(4):
                wg = wgb[g % 2]
                wgk = ("wgb", g % 2)
                ld("pool", wg[:, :, :], pwgrp_in[g].rearrange("(cc p) n -> p cc n", p=128), writes=[wgk])
                for dc in range(4):
                    for th in range(2):
                        bk = 2 + (pk2 % 4)
                        pk2 += 1
                        mm_group(bank(bk), [(wg[:, cc, dc * 128:(dc + 1) * 128], mixed[:, g * 4 + cc, th * 512:(th + 1) * 512]) for cc in range(4)],
                                 [wgk, "mixed"], [PSK[bk]])
                        fw.op("act", lambda e, bk=bk, g=g, dc=dc, th=th: e.activation(
                            out=ys[:, g * 4 + dc, th * 512:(th + 1) * 512], in_=bank(bk), func=AF.Copy, scale=pscale[:, g * 4 + dc:g * 4 + dc + 1]),
                            reads=[PSK[bk], "pscale"], writes=["ys"])
            ld("sp", ys_d[seg].rearrange("p (c t) -> p c t", c=KC), ys[:, :, :], reads=["ys"], writes=[("ys_d", seg)])
            fw.barrier()

    for l in range(2):
        if upto < 3 + l:
            break
        x_src = x_in if l == 0 else xl0_d
        x_dst = xl0_d if l == 0 else out_d
        wres_in = wout_in if l == 0 else pwout_in
        for seg in range(NSEG):
            if l == 1:
                pool_mixer(seg)
            with contextlib.ExitStack() as pl:
                hT = sb("hT_%d_%d" % (l, seg), [128, KC, T], BF16, stack=pl)
                gates = sb("gates_%d_%d" % (l, seg), [128, 8, NE], stack=pl)
                with contextlib.ExitStack() as pa_:
                    nm = "_%d_%d" % (l, seg)
                    Wres = sb("Wres" + nm, [128, KC, D], BF16, stack=pa_)
                    mixT = sb("mixT" + nm, [128, KC, T], BF16, stack=pa_)
                    xt = sb("xtl" + nm, [128, D], stack=pa_)
                    zt = sb("zt" + nm, [128, D], stack=pa_)
                    gb = sb("gb" + nm, [128, D], stack=pa_)
                    lngb = sb("lngb" + nm, [128, D], stack=pa_)
                    lnbb = sb("lnbb" + nm, [128, D], stack=pa_)
                    hTf = sb("hTf" + nm, [128, KC, 128], stack=pa_)
                    stats = sb("stats" + nm, [128, 4, 6], stack=pa_)
                    mv = sb("mv" + nm, [128, 2], stack=pa_)
                    rs_ = sb("rs" + nm, [128, 1], stack=pa_)
                    nmr = sb("nmr" + nm, [128, 1], stack=pa_)
                    lg = sb("lg" + nm, [128, NR], stack=pa_)
                    rsm = sb("rsm" + nm, [128, 64], stack=pa_)
                    ee = sb("ee" + nm, [128, 8], stack=pa_)
                    top8 = sb("top8" + nm, [128, 8], stack=pa_)
                    for q in range(4):
                        ld("pool", Wres[:, q * 4:(q + 1) * 4, :], wres_in[q * 512:(q + 1) * 512, :].rearrange("(kc p) n -> p kc n", p=128), writes=["Wres"])
                    bcast_rows(gb, grow[l][0], "gb")
                    bcast_rows(lngb, ln_g[2 * l:2 * l + 1, :], "lngb")
                    bcast_rows(lnbb, ln_b[2 * l:2 * l + 1, :], "lnbb")
                    if l == 0:
                        ld("sp", mixT[:, 0:8, :], att_d[seg].rearrange("p (h t) -> p h t", h=8), reads=[("att_d", seg)], writes=["mixT"])
                        ld("sp", mixT[:, 8:16, :], hg_d[seg].rearrange("p (h t) -> p h t", h=8), reads=[("hg_d", seg)], writes=["mixT"])
                    else:
                        ld("sp", mixT[:, :, :], ys_d[seg].rearrange("p (c t) -> p c t", c=KC), reads=[("ys_d", seg)], writes=["mixT"])
                    for tile in range(8):
                        r0 = seg * 1024 + tile * 128
                        yb = 0 if tile % 2 == 0 else 4
                        ld("sp", xt[:, :], x_src[r0:r0 + 128, :], reads=[("xsrc", l)], writes=["xtl"])
                        for nb in range(4):
                            mm_group(bank(yb + nb), [(mixT[:, kc, tile * 128:(tile + 1) * 128], Wres[:, kc, nb * 512:(nb + 1) * 512]) for kc in range(KC)],
                                     ["mixT", "Wres"], [PSK[yb + nb]])
                            fw.op("dve", lambda e, nb=nb, yb=yb: e.tensor_tensor(out=zt[:, nb * 512:(nb + 1) * 512], in0=bank(yb + nb),
                                                                                 in1=gb[:, nb * 512:(nb + 1) * 512], op=ALU.mult),
                                  reads=[PSK[yb + nb], "gb"], writes=["zt"])
                        fw.op("dve", lambda e: e.tensor_scalar(out=xt[:, :], in0=xt[:, :], scalar1=ALPHA, scalar2=None, op0=ALU.mult), reads=["xtl"], writes=["xtl"])
                        fw.op("dve", lambda e: e.tensor_add(out=zt[:, :], in0=xt[:, :], in1=zt[:, :]), reads=["xtl", "zt"], writes=["zt"])
                        layer_norm(zt, "zt", xt, "xtl", lngb, lnbb, (stats, mv, rs_, nmr), ["lngb", "lnbb"])
                        ld("sp", x1_d[l][r0:r0 + 128, :], xt[:, :], reads=["xtl"], writes=[("x1_d", l)])
                    fw.barrier()
                    for tile in range(8):
                        r0 = seg * 1024 + tile * 128
                        yb = 0 if tile % 2 == 0 else 4
                        ld("sp", xt[:, :], x1_d[l][r0:r0 + 128, :], reads=[("x1_d", l)], writes=["xtl"])

                        def f32copy(kc, src, pk, eng, l=l):
                            fw.op(eng,
                                  (lambda e: e.tensor_scalar(out=hTf[:, kc, :], in0=src, scalar1=mcol(l, 64 + kc, 0, True), scalar2=mcol(l, 48 + kc, 0),
                                                             op0=ALU.mult, op1=ALU.add)) if eng == "dve" else
                                  (lambda e: e.activation(out=hTf[:, kc, :], in_=src, func=AF.Identity, scale=mcol(l, 64 + kc, 0, True), bias=mcol(l, 48 + kc, 0))),
                                  reads=[pk, "modT", "mod1"], writes=["hTf"])
                        if not cfg.get("no_tr"):
                            trans_mod(xt, "xtl", hT, tile * 128, lambda kc, l=l: mcol(l, 64 + kc, 0, True), lambda kc, l=l: mcol(l, 48 + kc, 0), "hT",
                                      tbanks=((2, 3) if yb == 4 else (6, 7)), f32copy=(None if cfg.get('no_f32') else f32copy))
                        if cfg.get('no_router'):
                            fw.op('dve', lambda e, tile=tile: e.memset(gates[:, tile, :], 0.25), writes=['gates'])
                        else:
                            rb_ = 1 if yb == 4 else 5
                            mm_group(bank(rb_, NR), [(hTf[:, kc, :], rtw[:, l, kc, :]) for kc in range(KC)], ["hTf", "rtw"], [PSK[rb_]])
                            fw.op("dve", lambda e, rb_=rb_, l=l: e.tensor_tensor(out=lg[:, :], in0=bank(rb_, NR), in1=rtb[:, l, :], op=ALU.add),
                                  reads=[PSK[rb_], "rtb"], writes=["lg"])
                            R_ = ["lg", "rsm"]
                            fw.op("dve", lambda e: e.reduce_max(out=rsm[:, 0:1], in_=lg[:, 0:NG], axis=AX.X), reads=["lg"], writes=["rsm"])
                            fw.op("dve", lambda e: e.tensor_scalar(out=rsm[:, 1:2], in0=rsm[:, 0:1], scalar1=-1.0, scalar2=None, op0=ALU.mult), reads=R_, writes=["rsm"])
                            fw.op("act", lambda e: e.activation(out=rsm[:, 24:24 + NG], in_=lg[:, 0:NG], func=AF.Exp, bias=rsm[:, 1:2], accum_out=rsm[:, 2:3]),
                                  reads=R_, writes=["rsm"])
                            fw.op("dve", lambda e: e.reciprocal(out=rsm[:, 3:4], in_=rsm[:, 2:3]), reads=R_, writes=["rsm"])
                            fw.op("dve", lambda e: e.tensor_scalar(out=rsm[:, 8:8 + NG], in0=lg[:, 0:NG], scalar1=rsm[:, 0:1], scalar2=None, op0=ALU.is_ge),
                                  reads=R_, writes=["rsm"])
                            for g in range(NG):
                                if g == 0:
                                    fw.op("dve", lambda e: e.tensor_scalar(out=rsm[:, 16:16 + EPG], in0=lg[:, NG:NG + EPG], scalar1=rsm[:, 8:9], scalar2=None, op0=ALU.mult),
                                          reads=R_, writes=["rsm"])
                                else:
                                    fw.op("dve", lambda e, g=g: e.scalar_tensor_tensor(out=rsm[:, 16:16 + EPG], in0=lg[:, NG + g * EPG:NG + (g + 1) * EPG],
                                                                                       scalar=rsm[:, 8 + g:9 + g], in1=rsm[:, 16:16 + EPG], op0=ALU.mult, op1=ALU.add),
                                          reads=R_, writes=["rsm"])
                            fw.op("dve", lambda e: e.reduce_max(out=rsm[:, 4:5], in_=rsm[:, 16:16 + EPG], axis=AX.X), reads=R_, writes=["rsm"])
                            fw.op("dve", lambda e: e.tensor_scalar(out=rsm[:, 5:6], in0=rsm[:, 4:5], scalar1=-1.0, scalar2=None, op0=ALU.mult), reads=R_, writes=["rsm"])
                            fw.op("dve", lambda e: e.memset(ee[:, :], 0.0), writes=["ee"])
                            fw.op("act", lambda e: e.activation(out=ee[:, 0:EPG], in_=rsm[:, 16:16 + EPG], func=AF.Exp, bias=rsm[:, 5:6]), reads=R_ + ["ee"], writes=["ee"])
                            fw.op("dve", lambda e: e.max(out=top8[:, :], in_=ee[:, :]), reads=["ee"], writes=["top8"])
                            fw.op("dve", lambda e: e.tensor_add(out=rsm[:, 6:7], in0=top8[:, 0:1], in1=top8[:, 1:2]), reads=["top8", "rsm"], writes=["rsm"])
                            fw.op("dve", lambda e: e.reciprocal(out=rsm[:, 6:7], in_=rsm[:, 6:7]), reads=R_, writes=["rsm"])
                            fw.op("dve", lambda e: e.tensor_mul(out=rsm[:, 7:8], in0=rsm[:, 6:7], in1=rsm[:, 3:4]), reads=R_, writes=["rsm"])
                            fw.op("dve", lambda e: e.scalar_tensor_tensor(out=ee[:, :], in0=ee[:, :], scalar=top8[:, 1:2], in1=ee[:, :], op0=ALU.is_ge, op1=ALU.mult),
                                  reads=["ee", "top8"], writes=["ee"])
                            fw.op("dve", lambda e: e.tensor_scalar(out=ee[:, :], in0=ee[:, :], scalar1=rsm[:, 7:8], scalar2=None, op0=ALU.mult), reads=["ee", "rsm"], writes=["ee"])
                            for g in range(NG):
                                fw.op("dve", lambda e, g=g, tile=tile: e.tensor_scalar(out=gates[:, tile, g * EPG:(g + 1) * EPG], in0=ee[:, 0:EPG],
                                                                                       scalar1=rsm[:, 8 + g:9 + g], scalar2=None, op0=ALU.mult),
                                      reads=["ee", "rsm"], writes=["gates"])
                    fw.barrier()
                with contextlib.ExitStack() as pb_:
                    nm = "_%d_%d" % (l, seg)
                    yacc = sb("yacc" + nm, [128, 8, D], stack=pb_)
                    with contextlib.ExitStack() as pb2:
                        wr = [sb("wr%d" % i + nm, [128, KC * FF], BF16, stack=pb2) for i in range(4)]
                        aT = sb("aT" + nm, [128, 4, T], BF16, stack=pb2)
                        s1 = [sb("s1_%d" % i + nm, [128, 512], stack=pb2) for i in range(2)]
                        wn = 0
                        hb_ = 0
                        ybk = 0
                        if cfg.get('no_moe'):
                            for tile in range(8):
                                fw.op('dve', lambda e, tile=tile: e.memset(yacc[:, tile, :], 0.0), writes=[('yacc', tile)])
                        for ex in (range(0) if cfg.get('no_moe') else range(NE)):
                            slots = []
                            for wi, (src, shp) in enumerate(((w1_in[l, ex], "a"), (w3_in[l, ex], "a"), (w2_in[l, ex], "b"))):
                                i = wn % 4
                                wn += 1
                                if shp == "a":
                                    for q4 in range(4):
                                        ld("pool", wr[i][:, q4 * 4 * FF:(q4 + 1) * 4 * FF].rearrange("p (kc f) -> p kc f", kc=4),
                                           src[q4 * 512:(q4 + 1) * 512, :].rearrange("(kc p) f -> p kc f", p=128), writes=[("wr", i)])
                                else:
                                    ld("pool", wr[i][:, :].rearrange("p (fc n) -> p fc n", fc=4), src.rearrange("(fc p) n -> p fc n", p=128), writes=[("wr", i)])
                                slots.append(i)
                            w1s = wr[slots[0]][:, :].rearrange("p (kc f) -> p kc f", kc=KC)
                            w3s = wr[slots[1]][:, :].rearrange("p (kc f) -> p kc f", kc=KC)
                            w2s = wr[slots[2]][:, :].rearrange("p (fc n) -> p fc n", fc=4)
                            for fc in (range(0) if cfg.get('moe_part') in ('dma', 'y') else range(4)):
                                for th in range(2):
                                    b1 = (hb_ % 2) * 2
                                    hb_ += 1
                                    mm_group(bank(b1), [(w1s[:, kc, fc * 128:(fc + 1) * 128], hT[:, kc, th * 512:(th + 1) * 512]) for kc in range(KC)],
                                             [("wr", slots[0]), "hT"], [PSK[b1]])
                                    mm_group(bank(b1 + 1), [(w3s[:, kc, fc * 128:(fc + 1) * 128], hT[:, kc, th * 512:(th + 1) * 512]) for kc in range(KC)],
                                             [("wr", slots[1]), "hT"], [PSK[b1 + 1]])
                                    si = (b1 // 2)
                                    fw.op("act", lambda e, b1=b1, si=si: e.activation(out=s1[si][:, :], in_=bank(b1), func=AF.Silu), reads=[PSK[b1]], writes=[("s1", si)])
                                    fw.op("dve", lambda e, b1=b1, si=si, fc=fc, th=th: e.tensor_tensor(out=aT[:, fc, th * 512:(th + 1) * 512], in0=bank(b1 + 1), in1=s1[si][:, :], op=ALU.mult),
                                          reads=[("s1", si), PSK[b1 + 1]], writes=["aT"])
                            for tile in (range(0) if cfg.get('moe_part') in ('dma', 'h') else range(8)):
                                for nb in range(4):
                                    yb = 4 + (ybk % 4)
                                    ybk += 1
                                    mm_group(bank(yb), [(aT[:, fc, tile * 128:(tile + 1) * 128], w2s[:, fc, nb * 512:(nb + 1) * 512]) for fc in range(4)],
                                             ["aT", ("wr", slots[2])], [PSK[yb]])
                                    if ex == 0:
                                        fw.op("dve", lambda e, yb=yb, tile=tile, nb=nb, ex=ex: e.tensor_scalar(
                                            out=yacc[:, tile, nb * 512:(nb + 1) * 512], in0=bank(yb), scalar1=gates[:, tile, ex:ex + 1], scalar2=None, op0=ALU.mult),
                                            reads=[PSK[yb], "gates"], writes=[("yacc", tile)])
                                    else:
                                        fw.op("dve", lambda e, yb=yb, tile=tile, nb=nb, ex=ex: e.scalar_tensor_tensor(
                                            out=yacc[:, tile, nb * 512:(nb + 1) * 512], in0=bank(yb), scalar=gates[:, tile, ex:ex + 1],
                                            in1=yacc[:, tile, nb * 512:(nb + 1) * 512], op0=ALU.mult, op1=ALU.add),
                                            reads=[PSK[yb], "gates"], writes=[("yacc", tile)])
                        fw.barrier()
                    with contextlib.ExitStack() as pc_:
                        xt2v = sb("xt2" + nm, [128, D], stack=pc_)
                        gb2v = sb("gb2" + nm, [128, D], stack=pc_)
                        lngb2v = sb("lngb2" + nm, [128, D], stack=pc_)
                        lnbb2v = sb("lnbb2" + nm, [128, D], stack=pc_)
                        stats2v = sb("stats2" + nm, [128, 4, 6], stack=pc_)
                        mv2v = sb("mv2" + nm, [128, 2], stack=pc_)
                        rs2v = sb("rs2" + nm, [128, 1], stack=pc_)
                        nmr2v = sb("nmr2" + nm, [128, 1], stack=pc_)
                        bcast_rows(gb2v, grow[l][1], "gb")
                        bcast_rows(lngb2v, ln_g[2 * l + 1:2 * l + 2, :], "lngb")
                        bcast_rows(lnbb2v, ln_b[2 * l + 1:2 * l + 2, :], "lnbb")
                        for tile in (range(0) if cfg.get("no_c") else range(8)):
                            r0 = seg * 1024 + tile * 128
                            ld("sp", xt2v[:, :], x1_d[l][r0:r0 + 128, :], reads=[("x1_d", l)], writes=["xtl"])
                            yt = yacc[:, tile, :]
                            fw.op("dve", lambda e, yt=yt: e.tensor_tensor(out=yt, in0=yt, in1=gb2v[:, :], op=ALU.mult), reads=[("yacc", tile), "gb"], writes=[("yacc", tile)])
                            fw.op("dve", lambda e: e.tensor_scalar(out=xt2v[:, :], in0=xt2v[:, :], scalar1=ALPHA, scalar2=None, op0=ALU.mult), reads=["xtl"], writes=["xtl"])
                            fw.op("dve", lambda e, yt=yt: e.tensor_add(out=yt, in0=xt2v[:, :], in1=yt), reads=["xtl", ("yacc", tile)], writes=[("yacc", tile)])
                            layer_norm(yt, ("yacc", tile), xt2v, "xtl", lngb2v, lnbb2v, (stats2v, mv2v, rs2v, nmr2v), ["lngb", "lnbb"])
                            ld("sp", x_dst[r0:r0 + 128, :], xt2v[:, :], reads=["xtl"], writes=[("xsrc", l + 1)])
                            if dbg is not None and upto == 3 + l:
                                ld("sp", dbg[r0:r0 + 128, :], xt2v[:, :], reads=["xtl"])
                        fw.barrier()
        fw.barrier()

    fw.barrier()
    fw.emit(es)
    es.close()
    return nc


def rope_tables(seql):
    half = 64
    n_freq = 32
    pos = np.arange(seql)
    row = (pos // GRID_W).astype(np.float32)
    col = (pos % GRID_W).astype(np.float32)
    inv = (10000.0 ** (-np.arange(n_freq, dtype=np.float32) / n_freq)).astype(np.float32)
    cos = np.zeros((128, seql + 256), np.float32)
    sin = np.zeros((128, seql + 256), np.float32)
    for d in range(128):
        hf = d // 64
        j = d % 32
        isb = (d % 64) >= 32
        ang = (row if hf == 0 else col) * inv[j]
        cos[d, 128:128 + seql] = np.cos(ang)
        sin[d, 128:128 + seql] = np.sin(ang) * (1.0 if isb else -1.0)
    R = np.zeros((128, 128), np.float32)
    for d in range(128):
        partner = d + 32 if (d % 64) < 32 else d - 32
        R[partner, d] = 1.0
    return cos, sin, R


def make_in_maps(inp, cfg):
    nseg = cfg.get("nseg", 4)
    seql = nseg * 1024
    maps = []
    cos, sin, R = rope_tables(seql)
    a = np.arange(128)
    amask = np.zeros((128, 2, 128), np.float32)
    amask[:, 0, :] = (a[:, None] >= a[None, :])
    amask[:, 1, :] = (a[:, None] <= a[None, :])
    s64 = np.arange(64)
    hmask = np.zeros((64, 2, 8, 64), np.float32)
    hmask[:, 0] = (s64[:, None] <= s64[None, :])[:, None, :]
    hmask[:, 1] = (s64[:, None] >= s64[None, :])[:, None, :]
    resetp = np.ones((128, 1024), np.float32)
    resetp[:, ::64] = 0.0
    t = np.arange(seql)
    pinv = np.zeros((4, seql), np.float32)
    for gi, w in enumerate((2, 4, 8, 16)):
        lo = np.clip(t - w // 2, 0, seql)
        hi = np.clip(t + w - w // 2, 0, seql)
        pinv[gi] = 1.0 / (hi - lo)

    def pk(v, nch):
        return np.ascontiguousarray(np.asarray(v, np.float32).reshape(nch, 128).T)
    for b in range(2):
        m = {}
        m["x"] = inp["x"][b]
        m["ctx"] = inp["ctx"][b]
        cv = np.stack([inp["c"][b], inp["c_ctx"]], axis=-1)
        m["cvec"] = cv.reshape(KC, 128, 2).transpose(1, 0, 2).reshape(128, KC * 2)
        m["ada_w"] = inp["ada_w"]
        m["ada_b"] = inp["ada_b"].reshape(2, 96, 128).transpose(2, 0, 1).reshape(128, 192)
        m["ln_g"] = inp["ln_g"].reshape(4, D)
        m["ln_b"] = inp["ln_b"].reshape(4, D)
        m["ident"] = np.eye(128, dtype=np.float32)
        m["ropeR"] = R
        m["rcos"] = cos
        m["rsin"] = sin
        m["amask"] = amask.reshape(128, 256)
        m["hmask"] = hmask.reshape(64, 1024)
        m["resetp"] = resetp
        m["mix_w_in"] = inp["mix_w_in"][0]
        m["att_sink"] = inp["att_sink"][0].reshape(1, 8)
        lb = inp["hg_lb"].reshape(2, 3, 8, 128)
        m["hg_lb"] = lb.transpose(3, 0, 1, 2).reshape(128, 48)
        m["hg_norm_g"] = pk(inp["hg_norm_g"][0], 8)
        m["mix_w_out"] = inp["mix_w_out"][0]
        m["pool_w_in"] = inp["pool_w_in"][0]
        m["pool_w_grp"] = inp["pool_w_grp"][0]
        m["pool_scale"] = pk(inp["pool_scale"][0], 16)
        m["pool_w_out"] = inp["pool_w_out"][0]
        m["pinv"] = pinv.reshape(1, 4 * seql)
        m["rt_w"] = np.concatenate([inp["rt_group_w"], inp["rt_expert_w"]], axis=-1)
        m["rt_b"] = np.concatenate([inp["rt_group_b"], inp["rt_expert_b"]], axis=-1)
        m["moe_w1"] = inp["moe_w1"]
        m["moe_w3"] = inp["moe_w3"]
        m["moe_w2"] = inp["moe_w2"]
        maps.append({k: np.ascontiguousarray(v, dtype=np.float32) for k, v in m.items()})
    return maps


def kernel(**inputs):
    inp = {k: np.asarray(v) for k, v in inputs.items()}
    cfg = dict(nseg=4, ng=4, epg=8)
    nc = build(cfg)
    maps = make_in_maps(inp, cfg)
    res = run_bass_kernel_spmd(nc, maps, core_ids=[0, 1])
    out = np.stack([np.asarray(res.results[b]["out"], dtype=np.float32) for b in range(2)], axis=0)
    return out
```

```python
import numpy as np
import concourse.bass as bass
import concourse.mybir as mybir
from concourse.bass_utils import run_bass_kernel_spmd

F32 = mybir.dt.float32
BF16 = mybir.dt.bfloat16
ALU = mybir.AluOpType
AF = mybir.ActivationFunctionType
AX = mybir.AxisListType

D = 2048
NCORE = 8
T = 1024
TE = 1280
NCTX = 256
TA = TE + NCTX
KC = 16
GRID_W = 64
SEQ = 4096
ALPHA = 4.0 ** 0.25
LN_EPS = 1e-5
NORM_EPS = 1e-6
ATT_SCALE = 128.0 ** -0.5
NEXP = 32
FF = 512


import types


def _freeze(fn):
    if getattr(fn, "__closure__", None) is None:
        return fn
    cells = []
    for c in fn.__closure__:
        try:
            cells.append(types.CellType(c.cell_contents))
        except ValueError:
            cells.append(c)
    g = types.FunctionType(fn.__code__, fn.__globals__, fn.__name__, fn.__defaults__, tuple(cells))
    g.__kwdefaults__ = fn.__kwdefaults__
    return g


class FW:
    ENG = ("pe", "act", "dve", "pool", "sp")
    NDMA = 24

    def __init__(self, nc):
        self.nc = nc
        self.ops = []
        self.last_w = {}
        self.readers = {}
        self.pw = {}
        self.pending = {e: set() for e in self.ENG}
        self.dma_prev = {}
        self.n_dma = {}

    def _record(self, engine, fn, reads, writes, kind, pwrites=()):
        deps = set(self.pending[engine])
        self.pending[engine] = set()
        for k in reads:
            w = self.last_w.get(k)
            if w is not None:
                deps.add(w)
            deps.update(self.pw.get(k, ()))
        for k in writes:
            w = self.last_w.get(k)
            if w is not None:
                deps.add(w)
            deps.update(self.pw.get(k, ()))
            for r in self.readers.get(k, ()):
                deps.add(r)
        for k in pwrites:
            w = self.last_w.get(k)
            if w is not None:
                deps.add(w)
            for r in self.readers.get(k, ()):
                deps.add(r)
        oid = len(self.ops)
        op = dict(engine=engine, fn=_freeze(fn), deps=deps, kind=kind, marked=False, slot=None)
        if kind == "dma":
            k_ = self.n_dma.get(engine, 0)
            self.n_dma[engine] = k_ + 1
            slot = (engine, k_ % self.NDMA)
            prev = self.dma_prev.get(slot)
            if prev is not None:
                deps.add(prev)
            self.dma_prev[slot] = oid
            op["slot"] = slot
        self.ops.append(op)
        for k in reads:
            self.readers.setdefault(k, []).append(oid)
        for k in writes:
            self.last_w[k] = oid
            self.readers[k] = []
            self.pw[k] = []
        for k in pwrites:
            self.pw.setdefault(k, []).append(oid)
        return oid

    def op(self, engine, fn, reads=(), writes=(), pwrites=()):
        return self._record(engine, fn, list(reads), list(writes), "c", list(pwrites))

    def dma(self, engine, fn, reads=(), writes=()):
        return self._record(engine, fn, list(reads), list(writes), "dma")

    def cc(self, fn, reads=(), writes=()):
        return self._record("pool", fn, list(reads), list(writes), "cc")

    def barrier(self):
        allops = set()
        last = {}
        for i, o in enumerate(self.ops):
            if o["kind"] in ("dma", "cc"):
                allops.add(i)
            else:
                last[o["engine"]] = i
        allops |= set(last.values())
        for e in self.ENG:
            self.pending[e] |= allops

    def emit(self, nsem_ctx):
        nc = self.nc
        ops = self.ops
        for o in ops:
            for d in o["deps"]:
                ops[d]["marked"] = True
        for e in self.ENG:
            for d in self.pending[e]:
                ops[d]["marked"] = True
        cnt = {e: 0 for e in self.ENG}
        dcnt = {}
        ncc = 0
        for o in ops:
            if o["kind"] == "dma":
                s = o["slot"]
                dcnt[s] = dcnt.get(s, 0) + 16
                o["ev"] = (("d", s), dcnt[s])
            elif o["kind"] == "cc":
                o["ev"] = (("c", ncc), 1)
                ncc += 1
            elif o["marked"]:
                cnt[o["engine"]] += 1
                o["ev"] = (("e", o["engine"]), cnt[o["engine"]])
        sems = {}

        def sem(key):
            if key not in sems:
                sems[key] = nsem_ctx.enter_context(nc.semaphore("s_" + "_".join(str(x) for x in (key[1] if isinstance(key[1], tuple) else (key[1],))) + "_" + key[0]))
            return sems[key]

        for e in self.ENG:
            sem(("e", e))
        for s in sorted(dcnt):
            sem(("d", s))
        for c in range(ncc):
            sem(("c", c))
        streams = {e: [] for e in self.ENG}
        for i, o in enumerate(ops):
            streams[o["engine"]].append(i)
        final = {e: set(self.pending[e]) for e in self.ENG}

        def run(engine, eng):
            known = {}

            def wait_for(d):
                od = ops[d]
                if od["kind"] == "c" and od["engine"] == engine and engine in ("pe", "sp"):
                    return
                key, val = od["ev"]
                if known.get(key, 0) < val:
                    eng.wait_ge(sem(key), val)
                    known[key] = val

            for i in streams[engine]:
                o = ops[i]
                for d in sorted(o["deps"]):
                    wait_for(d)
                ins = o["fn"](eng)
                if o["kind"] in ("dma", "cc") or o["marked"]:
                    key, val = o["ev"]
                    if o["kind"] == "dma":
                        ins.then_inc(sem(key), 16)
                    else:
                        ins.then_inc(sem(key), 1)
            for d in sorted(final[engine]):
                wait_for(d)

        with nc.Block() as block:
            @block.sync
            def _(e):
                run("sp", e)

            @block.scalar
            def _(e):
                run("act", e)

            @block.vector
            def _(e):
                run("dve", e)

            @block.tensor
            def _(e):
                run("pe", e)

            @block.gpsimd
            def _(e):
                run("pool", e)


def build(cfg):
    import contextlib
    NSEG = cfg.get("nseg", 4)
    SEQL = NSEG * 1024
    NG = cfg.get("ng", 4)
    EPG = cfg.get("epg", 8)
    NE = NG * EPG
    NR = NG + NE
    upto = cfg.get("upto", 99)
    nc = bass.Bass("TRN2", target_bir_lowering=False)
    fw = FW(nc)
    es = contextlib.ExitStack()

    def din(name, shape, dt=F32):
        return nc.dram_tensor(name, list(shape), dt, kind="ExternalInput").ap()

    def dint(name, shape, dt=F32):
        return nc.dram_tensor(name, list(shape), dt).ap()

    sbn = {"n": 0}

    def sb(name, shape, dt=F32, stack=es):
        sbn["n"] += 1
        return stack.enter_context(nc.sbuf_tensor("sb%d_%s" % (sbn["n"], name), list(shape), dt))

    def ld(eng, out, in_, reads=(), writes=()):
        return fw.dma(eng, lambda e: e.dma_start(out=out, in_=in_), reads=reads, writes=writes)

    x_in = din("x", [SEQL, D])
    ctx_in = din("ctx", [NCTX, D])
    cvec = din("cvec", [128, KC * 2])
    ada_w = din("ada_w", [2, D, 6 * D])
    ada_b = din("ada_b", [128, 2 * 96])
    ln_g = din("ln_g", [4, D])
    ln_b = din("ln_b", [4, D])
    ident_in = din("ident", [128, 128])
    ropeR_in = din("ropeR", [128, 128])
    cos_in = din("rcos", [128, SEQL + 256])
    sin_in = din("rsin", [128, SEQL + 256])
    amask_in = din("amask", [128, 2 * 128])
    hmask_in = din("hmask", [64, 2 * 512])
    resetp_in = din("resetp", [128, 1024])
    win_in = din("mix_w_in", [D, 7168])
    sink_in = din("att_sink", [1, 8])
    hglb_in = din("hg_lb", [128, 2 * 3 * 8])
    hgng_in = din("hg_norm_g", [128, 8])
    wout_in = din("mix_w_out", [D, D])
    pwin_in = din("pool_w_in", [D, D])
    pwgrp_in = din("pool_w_grp", [4, 512, 512])
    pscale_in = din("pool_scale", [128, 16])
    pwout_in = din("pool_w_out", [D, D])
    pinv_in = din("pinv", [1, 4 * SEQL])
    rtw_in = din("rt_w", [2, D, NR])
    rtb_in = din("rt_b", [2, NR])
    w1_in = din("moe_w1", [2, NE, D, FF])
    w3_in = din("moe_w3", [2, NE, D, FF])
    w2_in = din("moe_w2", [2, NE, FF, D])
    out_d = nc.dram_tensor("out", [SEQL, D], F32, kind="ExternalOutput").ap()
    dbg = None
    if "dbg_shape" in cfg:
        dbg = nc.dram_tensor("dbg", list(cfg["dbg_shape"]), cfg.get("dbg_dt", F32), kind="ExternalOutput").ap()

    grow = [[dint("grow%d%d" % (l, w), [1, D]) for w in range(2)] for l in range(2)]
    att_d = [dint("att_d%d" % s, [128, 8 * 1024], BF16) for s in range(NSEG)]
    qe_d = [[dint("qe_d%d%d" % (d, s), [128, 8 * 1024], BF16) for s in range(NSEG)] for d in range(2)]
    ke_d = [[dint("ke_d%d%d" % (d, s), [128, 8 * 1024], BF16) for s in range(NSEG)] for d in range(2)]
    dec_d = [[dint("dec_d%d%d" % (d, s), [128, 8 * 16]) for s in range(NSEG)] for d in range(2)]
    vi_d = [dint("vi_d%d" % s, [64, 16 * 1024], BF16) for s in range(NSEG)]
    g_d = [dint("g_d%d" % s, [128, 8 * 1024], BF16) for s in range(NSEG)]
    of_d = [dint("of_d%d" % s, [128, 8 * 1024], BF16) for s in range(NSEG)]
    hg_d = [dint("hg_d%d" % s, [128, 8 * 1024], BF16) for s in range(NSEG)]
    x1_d = [dint("x1_d%d" % l, [SEQL, D]) for l in range(2)]
    xl0_d = dint("xl0_d", [SEQL, D])

    ps = es.enter_context(nc.psum_tensor("ps", [128, 4096], F32))

    def bank(b, n=512, o=0):
        return ps[:, b * 512 + o:b * 512 + o + n]

    ident_f = sb("ident_f", [128, 128])
    ident_b = sb("ident_b", [128, 128], BF16)
    ones_b = sb("ones_b", [128, 128], BF16)
    ropeR_b = sb("ropeR_b", [128, 128], BF16)
    amask_b = sb("amask_b", [128, 2, 128], BF16)
    hmask_b = sb("hmask_b", [64, 2, 512], BF16)
    resetp = sb("resetp", [128, 1024])
    modT = sb("modT", [128, 2, 96, 2])
    mod1 = sb("mod1", [128, 2, 96, 2])
    esink = sb("esink", [128, 8])
    lbt = sb("lbt", [128, 2, 8])
    omlb = sb("omlb", [128, 2, 8])
    normg = sb("normg", [128, 8])
    pscale = sb("pscale", [128, 16])
    kcT = sb("kcT", [128, 4, NCTX], BF16)
    vc_tok = sb("vc_tok", [128, 2, 512], BF16)
    S0 = [sb("S0_%d" % d, [128, 8, 128]) for d in range(2)]

    ld("sp", ident_f[:, :], ident_in, writes=["ident_f"])
    ld("pool", ident_b[:, :], ident_in, writes=["ident_b"])
    ld("pool", ropeR_b[:, :], ropeR_in, writes=["ropeR_b"])
    ld("pool", amask_b[:, :, :], amask_in.rearrange("p (w t) -> p w t", w=2), writes=["amask_b"])
    ld("pool", hmask_b[:, :, :], hmask_in.rearrange("p (w t) -> p w t", w=2), writes=["hmask_b"])
    ld("sp", resetp[:, :], resetp_in, writes=["resetp"])
    ld("sp", normg[:, :], hgng_in, writes=["normg"])
    ld("sp", pscale[:, :], pscale_in, writes=["pscale"])
    fw.op("dve", lambda e: e.memset(ones_b[:, :], 1.0), writes=["ones_b"])

    PSK = [("ps", b) for b in range(8)]
    rot = {"n": 0}

    def mm_group(out_ap, pairs, reads, writes):
        def f(e):
            ins = None
            n = len(pairs)
            for i, (l, r) in enumerate(pairs):
                ins = e.matmul(out_ap, lhsT=l, rhs=r, start=(i == 0), stop=(i == n - 1))
            return ins
        return fw.op("pe", f, reads=reads, writes=writes)

    with contextlib.ExitStack() as p0:
        cv = sb("cv", [128, KC, 2], stack=p0)
        scv = sb("scv", [128, KC, 2], stack=p0)
        adab = sb("adab", [128, 2, 96], stack=p0)
        wt = [sb("adaw%d" % i, [128, KC, 384], stack=p0) for i in range(2)]
        sk = sb("sk", [128, 8], stack=p0)
        lb3 = sb("lb3", [128, 2, 3, 8], stack=p0)
        lbs = sb("lbs", [128, 2, 8], stack=p0)
        tr16 = sb("tr16", [128, 16], stack=p0)
        tr16o = sb("tr16o", [16, 128], stack=p0)
        ld("sp", cv[:, :, :], cvec.rearrange("p (k v) -> p k v", v=2), writes=["cv"])
        ld("sp", adab[:, :, :], ada_b.rearrange("p (l c) -> p l c", l=2), writes=["adab"])
        ld("sp", sk[:, :], sink_in.partition_broadcast(128), writes=["sk"])
        ld("sp", lb3[:, :, :, :], hglb_in.rearrange("p (d l h) -> p d l h", d=2, l=3), writes=["lb3"])
        fw.op("act", lambda e: e.activation(out=scv[:, :, :], in_=cv[:, :, :], func=AF.Silu), reads=["cv"], writes=["scv"])
        fw.op("act", lambda e: e.activation(out=esink[:, :], in_=sk[:, :], func=AF.Exp), reads=["sk"], writes=["esink"])
        fw.op("act", lambda e: e.activation(out=lb3[:, :, :, :], in_=lb3[:, :, :, :], func=AF.Exp), reads=["lb3"], writes=["lb3"])
        fw.op("dve", lambda e: e.tensor_add(out=lbs[:, :, :], in0=lb3[:, :, 0, :], in1=lb3[:, :, 1, :]), reads=["lb3"], writes=["lbs"])
        fw.op("dve", lambda e: e.tensor_add(out=lbs[:, :, :], in0=lbs[:, :, :], in1=lb3[:, :, 2, :]), reads=["lb3", "lbs"], writes=["lbs"])
        fw.op("dve", lambda e: e.reciprocal(out=lbs[:, :, :], in_=lbs[:, :, :]), reads=["lbs"], writes=["lbs"])
        fw.op("dve", lambda e: e.tensor_mul(out=lbt[:, :, :], in0=lb3[:, :, 0, :], in1=lbs[:, :, :]), reads=["lb3", "lbs"], writes=["lbt"])
        fw.op("dve", lambda e: e.tensor_scalar(out=omlb[:, :, :], in0=lbt[:, :, :], scalar1=-1.0, scalar2=1.0,
                                               op0=ALU.mult, op1=ALU.add), reads=["lbt"], writes=["omlb"])
        pi = 0
        for l in range(2):
            for q in range(32):
                w = wt[pi % 2]
                wk = ("adaw", pi % 2)
                src = ada_w[l, :, q * 384:(q + 1) * 384].rearrange("(kc p) j -> p kc j", p=128)
                ld("sp", w[:, :, :], src, writes=[wk])
                for j3 in range(3):
                    c = q * 3 + j3
                    bk = (pi * 3 + j3) % 8
                    pb = bank(bk, 2)
                    mm_group(pb, [(w[:, kc, j3 * 128:(j3 + 1) * 128], scv[:, kc, :]) for kc in range(KC)],
                             [wk, "scv"], [PSK[bk]])
                    fw.op("dve", lambda e, pb=pb, l=l, c=c: e.tensor_scalar(
                        out=modT[:, l, c, :], in0=pb, scalar1=adab[:, l, c:c + 1], scalar2=None, op0=ALU.add),
                        reads=[PSK[bk], "adab"], writes=["modT"])
                pi += 1
        fw.op("dve", lambda e: e.tensor_scalar_add(out=mod1[:, :, :, :], in0=modT[:, :, :, :], scalar1=1.0),
              reads=["modT"], writes=["mod1"])
        for l in range(2):
            for w_, c0 in enumerate((32, 80)):
                fw.op("dve", lambda e, l=l, c0=c0: e.tensor_copy(out=tr16[:, :], in_=modT[:, l, c0:c0 + 16, 0]),
                      reads=["modT"], writes=["tr16"])
                fw.op("pe", lambda e: e.transpose(out=bank(0, 128)[0:16, :], in_=tr16[:, :], identity=ident_f[:, :]),
                      reads=["tr16", "ident_f"], writes=[PSK[0]])
                fw.op("dve", lambda e: e.tensor_copy(out=tr16o[:, :], in_=bank(0, 128)[0:16, :]), reads=[PSK[0]], writes=["tr16o"])
                ld("sp", grow[l][w_].rearrange("o (c p) -> (o c) p", p=128), tr16o[:, :], reads=["tr16o"], writes=[("grow", l, w_)])
        fw.barrier()

    def mcol(l, c, v=0, one=False):
        t = mod1 if one else modT
        return t[:, l, c, v:v + 1]


    def psbf(b):
        return ps[:, b * 512:(b + 1) * 512].bitcast(BF16)

    def bc(ap2, n):
        return ap2.unsqueeze(2).to_broadcast([128, 8, n])

    def hg_pass(d, qeT, keT, dec, vi, nch, S, Sk, Sd, Sbf, ktok, scb, ocb, kkey, vkey):
        order = range(nch) if d == 0 else range(nch - 1, -1, -1)
        PSS = ps[:, 6 * 512:8 * 512]
        psk = [PSK[6], PSK[7]]
        if d == 0 and qeT is not None:
            fw.op("act", lambda e: e.copy(out=Sbf[:, :, :], in_=S[:, :, :]), reads=[Sk], writes=["Sbf"])
        for c in order:
            s0 = 0 if c % 2 == 0 else 3
            cs = slice(c * 64, (c + 1) * 64)

            def tr(e, c=c, s0=s0, cs=cs):
                ins = None
                for h in range(8):
                    ins = e.transpose(out=psbf(s0)[0:64, h * 128:(h + 1) * 128], in_=keT[:, h, cs], identity=ident_b[:, :])
                return ins
            fw.op("pe", tr, reads=[kkey + "ke", "ident_b"], writes=[PSK[s0]])
            fw.op("act", lambda e, s0=s0: e.copy(out=ktok[:, :, :], in_=psbf(s0)[0:64, :].rearrange("p (h k) -> p h k", h=8)),
                  reads=[PSK[s0]], writes=["ktok"])
            if qeT is not None:
                def sc(e, s0=s0, cs=cs):
                    ins = None
                    for h in range(8):
                        ins = e.matmul(bank(s0 + 1)[0:64, h * 64:(h + 1) * 64], lhsT=keT[:, h, cs], rhs=qeT[:, h, cs], start=True, stop=True)
                    return ins
                fw.op("pe", sc, reads=[kkey + "ke", kkey + "qe"], writes=[PSK[s0 + 1]])
                fw.op("dve", lambda e, s0=s0: e.tensor_tensor(out=scb[:, :], in0=bank(s0 + 1)[0:64, :], in1=hmask_b[:, d, :], op=ALU.mult),
                      reads=[PSK[s0 + 1], "hmask_b"], writes=["scb"])
            if d == 1:
                fw.op("dve", lambda e, c=c: e.tensor_tensor(out=Sd[:, :, :], in0=S[:, :, :], in1=bc(dec[:, :, c], 128), op=ALU.mult),
                      reads=[Sk, kkey + "dec"], writes=["Sd"])
                if qeT is not None:
                    fw.op("act", lambda e: e.copy(out=Sbf[:, :, :], in_=Sd[:, :, :]), reads=["Sd"], writes=["Sbf"])
            if qeT is not None:
                def om(e, c=c, s0=s0, cs=cs):
                    ins = None
                    for h in range(8):
                        o = bank(s0 + 2)[:, h * 64:(h + 1) * 64]
                        e.matmul(o, lhsT=Sbf[:, h, :], rhs=qeT[:, h, cs], start=True, stop=False)
                        ins = e.matmul(o, lhsT=vi[:, c, h * 128:(h + 1) * 128], rhs=scb[:, h * 64:(h + 1) * 64], start=False, stop=True)
                    return ins
                fw.op("pe", om, reads=["Sbf", kkey + "qe", vkey, "scb"], writes=[PSK[s0 + 2]])
                ocb(c, bank(s0 + 2), PSK[s0 + 2])
            Ssrc = S if d == 0 else Sd

            def stm(e, c=c, Ssrc=Ssrc):
                ins = None
                for h in range(8):
                    o = PSS[:, h * 128:(h + 1) * 128]
                    e.matmul(o, lhsT=ident_f[:, :], rhs=Ssrc[:, h, :], start=True, stop=False)
                    ins = e.matmul(o, lhsT=ktok[:, h, :], rhs=vi[:, c, h * 128:(h + 1) * 128], start=False, stop=True)
                return ins
            fw.op("pe", stm, reads=[Sk, "Sd", "ktok", vkey, "ident_f"], writes=psk)
            for hf in range(2):
                src = PSS[:, hf * 512:(hf + 1) * 512].rearrange("p (h v) -> p h v", h=4)
                dst = S[:, hf * 4:(hf + 1) * 4, :]
                if d == 0:
                    fw.op("dve", lambda e, src=src, dst=dst, c=c, hf=hf: e.tensor_tensor(
                        out=dst, in0=src, in1=dec[:, hf * 4:(hf + 1) * 4, c].unsqueeze(2).to_broadcast([128, 4, 128]), op=ALU.mult),
                        reads=[psk[hf], kkey + "dec"], writes=[Sk])
                else:
                    fw.op("act", lambda e, src=src, dst=dst: e.copy(out=dst, in_=src), reads=[psk[hf]], writes=[Sk])
            if d == 0 and qeT is not None:
                fw.op("act", lambda e: e.copy(out=Sbf[:, :, :], in_=S[:, :, :]), reads=[Sk], writes=["Sbf"])

    tcnt = {"n": 0}

    def trans_mod(xt, xk, dstT, col0, scale_fn, bias_fn, wkey, tbanks=(0, 1), f32copy=None, npart=128):
        for q in range(4):
            bk = tbanks[tcnt["n"] % 2]
            tcnt["n"] += 1

            def tr(e, q=q, bk=bk):
                ins = None
                for j in range(4):
                    kc = q * 4 + j
                    ins = e.transpose(out=bank(bk, npart, j * 128), in_=xt[0:npart, kc * 128:(kc + 1) * 128], identity=ident_f[0:npart, 0:npart])
                return ins
            fw.op("pe", tr, reads=[xk, "ident_f"], writes=[PSK[bk]])
            for j in range(4):
                kc = q * 4 + j
                src = bank(bk, npart, j * 128)
                dst = dstT[:, kc, col0:col0 + npart]
                if (q % 2) == 0:
                    fw.op("act", lambda e, src=src, dst=dst, kc=kc: e.activation(
                        out=dst, in_=src, func=AF.Identity, scale=scale_fn(kc), bias=bias_fn(kc)),
                        reads=[PSK[bk], "modT", "mod1"], pwrites=[wkey])
                else:
                    fw.op("dve", lambda e, src=src, dst=dst, kc=kc: e.tensor_scalar(
                        out=dst, in0=src, scalar1=scale_fn(kc), scalar2=bias_fn(kc), op0=ALU.mult, op1=ALU.add),
                        reads=[PSK[bk], "modT", "mod1"], pwrites=[wkey])
                if f32copy is not None:
                    f32copy(kc, src, PSK[bk], "act" if (q % 2) == 0 else "dve")

    with contextlib.ExitStack() as p1:
        hxT = sb("hxT", [128, KC, TE], BF16, stack=p1)
        hcT = sb("hcT", [128, KC, NCTX], BF16, stack=p1)
        wbuf = [sb("wbuf%d" % i, [128, KC, 256], BF16, stack=p1) for i in range(3)]
        wcnt = {"n": 0}
        pcnt = {"n": 0}
        PB = (2, 3, 4, 5)

        def load_wpiece(col0, ncols=256):
            i = wcnt["n"] % 3
            wcnt["n"] += 1
            src = win_in[:, col0:col0 + ncols].rearrange("(kc p) j -> p kc j", p=128)
            ld("pool", wbuf[i][:, :, 0:ncols], src, writes=[("wbuf", i)])
            return wbuf[i], ("wbuf", i)

        def proj_fm(wb, wk, cb, srcT, skeys, tok0, ntok, evac):
            bk = PB[pcnt["n"] % 4]
            pcnt["n"] += 1
            mm_group(bank(bk, ntok), [(wb[:, kc, cb * 128:(cb + 1) * 128], srcT[:, kc, tok0:tok0 + ntok]) for kc in range(KC)],
                     [wk] + list(skeys), [PSK[bk]])
            evac(bank(bk, ntok), PSK[bk])

        def proj_tm(wb, wk, ncols, srcT, skeys, tok0, nt, evac):
            bk = PB[pcnt["n"] % 4]
            pcnt["n"] += 1
            mm_group(bank(bk, ncols)[0:nt, :], [(srcT[:, kc, tok0:tok0 + nt], wb[:, kc, 0:ncols]) for kc in range(KC)],
                     [wk] + list(skeys), [PSK[bk]])
            evac(bank(bk, ncols)[0:nt, :], PSK[bk])

        for seg in range(NSEG):
            pxt = contextlib.ExitStack()
            xts = [sb("xt%d_%d" % (i, seg), [128, D], stack=pxt) for i in range(2)]
            for tt in range(10):
                g0 = seg * 1024 - 128 + tt * 128
                if g0 < 0 or g0 >= SEQL:
                    fw.op("pool", lambda e, tt=tt: e.memset(hxT[:, :, tt * 128:(tt + 1) * 128], 0.0), writes=[("hxT", tt)])
                    continue
                xt = xts[tt % 2]
                xk = ("xt", tt % 2)
                ld("sp", xt[:, :], x_in[g0:g0 + 128, :], writes=[xk])
                trans_mod(xt, xk, hxT, tt * 128, lambda kc: mcol(0, 16 + kc, 0, True), lambda kc: mcol(0, kc, 0), ("hxT", tt))
            if seg == 0:
                for tt in range(2):
                    xt = xts[tt % 2]
                    xk = ("xt", tt % 2)
                    ld("sp", xt[:, :], ctx_in[tt * 128:(tt + 1) * 128, :], writes=[xk])
                    trans_mod(xt, xk, hcT, tt * 128, lambda kc: mcol(0, 16 + kc, 1, True), lambda kc: mcol(0, kc, 1), ("hcT", tt))
            fw.barrier()
            pxt.close()
            HXK = [("hxT", tt) for tt in range(10)]
            HCK = [("hcT", 0), ("hcT", 1)]

            def hx_keys(tok0, ntok):
                return [("hxT", t) for t in range(tok0 // 128, (tok0 + ntok + 127) // 128)]

            with contextlib.ExitStack() as pa:
                qT = sb("qT", [128, 8, T], BF16, stack=pa)
                kT = sb("kT", [128, 4, TE], BF16, stack=pa)
                v_tok = sb("v_tok", [128, 10, 512], BF16, stack=pa)
                attT = sb("attT", [128, 8, T], BF16, stack=pa)
                rc = sb("rc", [128, TE], stack=pa)
                rs = sb("rs", [128, TE], stack=pa)
                qraw = [sb("qraw%d" % i, [128, 512], BF16, stack=pa) for i in range(2)]
                rt1 = [sb("rt1_%d" % i, [128, 512], stack=pa) for i in range(2)]
                rt2 = [sb("rt2_%d" % i, [128, 512], stack=pa) for i in range(2)]
                pT = [sb("pT%d" % i, [128, 5 * 256], BF16, stack=pa) for i in range(2)]
                den = [sb("den%d" % i, [128, 256], stack=pa) for i in range(2)]
                ld("sp", rc[:, :], cos_in[:, seg * 1024:seg * 1024 + TE], writes=["rc"])
                ld("sp", rs[:, :], sin_in[:, seg * 1024:seg * 1024 + TE], writes=["rs"])
                rcnt = {"n": 0}

                def rope_evac(dst, e0, ntok):
                    def evac(pb, pk):
                        i = rcnt["n"] % 2
                        rcnt["n"] += 1
                        rb = 6 + i
                        qr, t1, t2 = qraw[i], rt1[i], rt2[i]
                        fw.op("act", lambda e: e.copy(out=qr[:, 0:ntok], in_=pb), reads=[pk], writes=[("qraw", i)])
                        fw.op("pe", lambda e: e.matmul(bank(rb, ntok), lhsT=ropeR_b[:, :], rhs=qr[:, 0:ntok], start=True, stop=True),
                              reads=[("qraw", i), "ropeR_b"], writes=[PSK[rb]])
                        fw.op("dve", lambda e: e.tensor_mul(out=t1[:, 0:ntok], in0=qr[:, 0:ntok], in1=rc[:, e0:e0 + ntok]),
                              reads=[("qraw", i), "rc"], writes=[("rt1", i)])
                        fw.op("dve", lambda e: e.tensor_mul(out=t2[:, 0:ntok], in0=bank(rb, ntok), in1=rs[:, e0:e0 + ntok]),
                              reads=[PSK[rb], "rs"], writes=[("rt2", i)])
                        fw.op("dve", lambda e: e.tensor_add(out=dst, in0=t1[:, 0:ntok], in1=t2[:, 0:ntok]),
                              reads=[("rt1", i), ("rt2", i)], writes=["qk"])
                    return evac

                for pc in range(4):
                    wb, wk = load_wpiece(pc * 256)
                    for hb in range(2):
                        for th in range(2):
                            proj_fm(wb, wk, hb, hxT, hx_keys(128 + th * 512, 512), 128 + th * 512, 512,
                                    rope_evac(qT[:, pc * 2 + hb, th * 512:(th + 1) * 512], 128 + th * 512, 512))
                for pc in range(2):
                    wb, wk = load_wpiece(1024 + pc * 256)
                    for hb in range(2):
                        for (t0, nt) in ((0, 512), (512, 512), (1024, 256)):
                            proj_fm(wb, wk, hb, hxT, hx_keys(t0, nt), t0, nt, rope_evac(kT[:, pc * 2 + hb, t0:t0 + nt], t0, nt))
                        if seg == 0:
                            h = pc * 2 + hb
                            proj_fm(wb, wk, hb, hcT, HCK, 0, NCTX,
                                    lambda pb, pk, h=h: fw.op("act", lambda e: e.copy(out=kcT[:, h, :], in_=pb), reads=[pk], writes=["kcT"]))
                for pc in range(2):
                    wb, wk = load_wpiece(1536 + pc * 256)
                    for tt in range(10):
                        proj_tm(wb, wk, 256, hxT, [("hxT", tt)], tt * 128, 128,
                                lambda pb, pk, tt=tt, pc=pc: fw.op("act" if tt % 2 else "dve", (lambda e: e.copy(out=v_tok[:, tt, pc * 256:(pc + 1) * 256], in_=pb)) if tt % 2 else
                                                                   (lambda e: e.tensor_copy(out=v_tok[:, tt, pc * 256:(pc + 1) * 256], in_=pb)), reads=[pk], pwrites=["v_tok"]))
                    if seg == 0:
                        for tt in range(2):
                            proj_tm(wb, wk, 256, hcT, HCK, tt * 128, 128,
                                    lambda pb, pk, tt=tt, pc=pc: fw.op("act", lambda e: e.copy(out=vc_tok[:, tt, pc * 256:(pc + 1) * 256], in_=pb), reads=[pk], writes=["vc_tok"]))
                ai = 0
                for h in range(4):
                    for n in range(8):
                        blocks = []
                        if not (seg == 0 and n == 0):
                            blocks.append(("l", n, 0))
                        blocks.append(("l", n + 1, None))
                        if not (seg == NSEG - 1 and n == 7):
                            blocks.append(("l", n + 2, 1))
                        blocks.append(("c", 0, None))
                        blocks.append(("c", 1, None))
                        nb = len(blocks)
                        st = ai % 2
                        ai += 1
                        sb0 = 0 if st == 0 else 4
                        sk_ = [PSK[sb0], PSK[sb0 + 1], PSK[sb0 + 2]]
                        ok_ = PSK[sb0 + 3]
                        rhs_q = qT[:, 2 * h:2 * h + 2, n * 128:(n + 1) * 128]

                        def scores(e, blocks=blocks, sb0=sb0, h=h, rhs_q=rhs_q):
                            ins = None
                            for bi, (kind, idx, _) in enumerate(blocks):
                                lhs = kT[:, h, idx * 128:(idx + 1) * 128] if kind == "l" else kcT[:, h, idx * 128:(idx + 1) * 128]
                                o = ps[:, sb0 * 512 + bi * 256: sb0 * 512 + (bi + 1) * 256]
                                ins = e.matmul(o, lhsT=lhs, rhs=rhs_q, start=True, stop=True)
                            return ins
                        fw.op("pe", scores, reads=["qk", "kcT"], writes=sk_)
                        p = pT[st]
                        pk_ = ("pT", st)
                        fw.op("act", lambda e, p=p, sb0=sb0, nb=nb: e.activation(
                            out=p[:, 0:nb * 256], in_=ps[:, sb0 * 512: sb0 * 512 + nb * 256], func=AF.Exp, scale=ATT_SCALE),
                            reads=sk_, writes=[pk_])
                        for bi, (kind, idx, mk) in enumerate(blocks):
                            if mk is not None:
                                pv = p[:, bi * 256:(bi + 1) * 256].rearrange("p (g t) -> p g t", g=2)
                                fw.op("dve", lambda e, pv=pv, mk=mk: e.tensor_tensor(
                                    out=pv, in0=pv, in1=amask_b[:, mk, :].unsqueeze(1).to_broadcast([128, 2, 128]), op=ALU.mult),
                                    reads=[pk_, "amask_b"], writes=[pk_])

                        def pv_mm(e, blocks=blocks, sb0=sb0, h=h, p=p, nb=nb):
                            ins = None
                            oo = ps[:, (sb0 + 3) * 512:(sb0 + 3) * 512 + 256]
                            os_ = ps[:, (sb0 + 3) * 512 + 256:(sb0 + 3) * 512 + 512]
                            for bi, (kind, idx, _) in enumerate(blocks):
                                vv = v_tok[:, idx, h * 128:(h + 1) * 128] if kind == "l" else vc_tok[:, idx, h * 128:(h + 1) * 128]
                                ins = e.matmul(oo, lhsT=vv, rhs=p[:, bi * 256:(bi + 1) * 256], start=(bi == 0), stop=(bi == nb - 1))
                            for bi in range(nb):
                                ins = e.matmul(os_, lhsT=ones_b[:, :], rhs=p[:, bi * 256:(bi + 1) * 256], start=(bi == 0), stop=(bi == nb - 1))
                            return ins
                        fw.op("pe", pv_mm, reads=[pk_, "v_tok", "vc_tok", "ones_b"], writes=[ok_])
                        dn = den[st]
                        dk = ("den", st)
                        for g in range(2):
                            fw.op("dve", lambda e, g=g, dn=dn, sb0=sb0, h=h: e.tensor_scalar(
                                out=dn[:, g * 128:(g + 1) * 128], in0=ps[:, (sb0 + 3) * 512 + 256 + g * 128:(sb0 + 3) * 512 + 256 + (g + 1) * 128],
                                scalar1=esink[:, 2 * h + g:2 * h + g + 1], scalar2=None, op0=ALU.add), reads=[ok_, "esink"], writes=[dk])
                        fw.op("dve", lambda e, dn=dn: e.reciprocal(out=dn[:, :], in_=dn[:, :]), reads=[dk], writes=[dk])
                        fw.op("dve", lambda e, dn=dn, sb0=sb0, h=h, n=n: e.tensor_tensor(
                            out=attT[:, 2 * h:2 * h + 2, n * 128:(n + 1) * 128],
                            in0=ps[:, (sb0 + 3) * 512:(sb0 + 3) * 512 + 256].rearrange("p (g t) -> p g t", g=2),
                            in1=dn[:, :].rearrange("p (g t) -> p g t", g=2), op=ALU.mult), reads=[ok_, dk], writes=["attT"])
                ld("sp", att_d[seg].rearrange("p (h t) -> p h t", h=8), attT[:, :, :], reads=["attT"], writes=[("att_d", seg)])
                if upto == 1 and seg == 0:
                    ld("pool", dbg.rearrange("p (h t) -> p h t", h=8), attT[:, :, :], reads=["attT"])
                fw.barrier()
            if upto == 1:
                continue

            with contextlib.ExitStack() as ph:
                qhT = sb("qhT", [128, 8, T], BF16, stack=ph)
                tf = sb("tf", [128, T], stack=ph)
                tk = sb("tk", [128, T], stack=ph)
                tg = sb("tg", [128, T], stack=ph)
                tP = sb("tP", [128, T], stack=ph)
                te1 = sb("te1", [128, T], stack=ph)
                te2 = sb("te2", [128, T], stack=ph)
                qe_s = [sb("qe_s%d" % i, [128, T], BF16, stack=ph) for i in range(2)]
                ke_s = [sb("ke_s%d" % i, [128, T], BF16, stack=ph) for i in range(2)]
                decst = sb("decst", [128, 8, 16], stack=ph)
                vi64 = sb("vi64", [64, 16, 1024], BF16, stack=ph)
                scnt = {"n": 0}
                for pc in range(4):
                    wb, wk = load_wpiece(2048 + pc * 256)
                    for hb in range(2):
                        for th in range(2):
                            dst = qhT[:, pc * 2 + hb, th * 512:(th + 1) * 512]
                            proj_fm(wb, wk, hb, hxT, hx_keys(128 + th * 512, 512), 128 + th * 512, 512,
                                    lambda pb, pk, dst=dst: fw.op("act", lambda e: e.activation(out=dst, in_=pb, func=AF.Silu), reads=[pk], writes=["qhT"]))

                def prep(d, h, wb, wk, hb, srcT, skeys_fn, tok0, ntok, qsrc, qe_out, ke_out, dec_out, okey):
                    nch = ntok // 64
                    for t0 in range(0, ntok, 512):
                        nt = min(512, ntok - t0)
                        proj_fm(wb, wk, hb, srcT, skeys_fn(tok0 + t0, nt), tok0 + t0, nt,
                                lambda pb, pk, t0=t0, nt=nt: fw.op("act", lambda e: e.activation(out=tf[:, t0:t0 + nt], in_=pb, func=AF.Sigmoid), reads=[pk], writes=["tf"]))
                    fw.op("dve", lambda e: e.tensor_scalar(out=tf[:, 0:ntok], in0=tf[:, 0:ntok], scalar1=omlb[:, d, h:h + 1], scalar2=lbt[:, d, h:h + 1],
                                                           op0=ALU.mult, op1=ALU.add), reads=["tf", "omlb", "lbt"], writes=["tf"])
                    fw.op("act", lambda e: e.activation(out=tg[:, 0:ntok], in_=tf[:, 0:ntok], func=AF.Ln), reads=["tf"], writes=["tg"])
                    fw.op("pool", lambda e: e.tensor_scalar(out=tk[:, 0:ntok], in0=tf[:, 0:ntok], scalar1=-1.0, scalar2=1.0, op0=ALU.mult, op1=ALU.add),
                          reads=["tf"], writes=["tk"])
                    fw.op("dve", lambda e: e.tensor_tensor_scan(out=tP[:, 0:ntok], data0=resetp[:, 0:ntok], data1=tg[:, 0:ntok], initial=0.0,
                                                                op0=ALU.mult, op1=ALU.add), reads=["tg", "resetp"], writes=["tP"])
                    pend = tP[:, 0:ntok].rearrange("p (c j) -> p c j", j=64)[:, :, 63]
                    if d == 0:
                        fw.op("act", lambda e: e.activation(out=te1[:, 0:ntok], in_=tP[:, 0:ntok], func=AF.Exp), reads=["tP"], writes=["te1"])
                        fw.op("act", lambda e: e.activation(out=te2[:, 0:ntok], in_=tP[:, 0:ntok], func=AF.Exp, scale=-1.0), reads=["tP"], writes=["te2"])
                    else:
                        fw.op("pool", lambda e: e.tensor_sub(out=tg[:, 0:ntok], in0=tP[:, 0:ntok], in1=tg[:, 0:ntok]), reads=["tP", "tg"], writes=["tg"])
                        fw.op("act", lambda e: e.activation(out=te1[:, 0:ntok], in_=tg[:, 0:ntok], func=AF.Exp, scale=-1.0), reads=["tg"], writes=["te1"])
                        fw.op("act", lambda e: e.activation(out=te2[:, 0:ntok], in_=tg[:, 0:ntok], func=AF.Exp), reads=["tg"], writes=["te2"])
                    fw.op("act", lambda e: e.activation(out=dec_out[:, h, 0:nch], in_=pend, func=AF.Exp), reads=["tP"], writes=[okey + "dec"])
                    if qsrc is not None:
                        fw.op("dve", lambda e: e.tensor_mul(out=qe_out, in0=qsrc, in1=te1[:, 0:ntok]), reads=["qhT", "te1"], writes=[okey + "qe"])
                    fw.op("pool", lambda e: e.tensor_mul(out=ke_out, in0=tk[:, 0:ntok], in1=te2[:, 0:ntok]), reads=["tk", "te2"], writes=[okey + "ke"])

                if seg == 0:
                    kce = [sb("kce%d" % d, [128, 8, NCTX], BF16, stack=ph) for d in range(2)]
                    decc = [sb("decc%d" % d, [128, 8, 4], stack=ph) for d in range(2)]
                    vic = sb("vic", [64, 4, 1024], BF16, stack=ph)
                for d in range(2):
                    for pc in range(4):
                        wb, wk = load_wpiece(3072 + d * 1024 + pc * 256)
                        for hb in range(2):
                            h = pc * 2 + hb
                            i = scnt["n"] % 2
                            scnt["n"] += 1
                            prep(d, h, wb, wk, hb, hxT, hx_keys, 128, T, qhT[:, h, :], qe_s[i][:, :], ke_s[i][:, :], decst, "st%d" % i)
                            ld("sp", qe_d[d][seg][:, h * 1024:(h + 1) * 1024], qe_s[i][:, :], reads=["st%dqe" % i], writes=[("qe_d", d, seg)])
                            ld("sp", ke_d[d][seg][:, h * 1024:(h + 1) * 1024], ke_s[i][:, :], reads=["st%dke" % i], writes=[("ke_d", d, seg)])
                            if seg == 0:
                                prep(d, h, wb, wk, hb, hcT, lambda a, b: HCK, 0, NCTX, None, None, kce[d][:, h, :], decc[d], "cx%d" % d)
                    ld("sp", dec_d[d][seg].rearrange("p (h c) -> p h c", h=8), decst[:, :, :], reads=["st0dec", "st1dec"], writes=[("dec_d", d, seg)])
                for pc in range(4):
                    wb, wk = load_wpiece(5120 + pc * 256)
                    for c in range(16):
                        proj_tm(wb, wk, 256, hxT, hx_keys(128 + c * 64, 64), 128 + c * 64, 64,
                                lambda pb, pk, c=c, pc=pc: fw.op("act" if c % 2 else "dve", (lambda e: e.copy(out=vi64[:, c, pc * 256:(pc + 1) * 256], in_=pb)) if c % 2 else
                                                                 (lambda e: e.tensor_copy(out=vi64[:, c, pc * 256:(pc + 1) * 256], in_=pb)), reads=[pk], pwrites=["vi64"]))
                    if seg == 0:
                        for c in range(4):
                            proj_tm(wb, wk, 256, hcT, HCK, c * 64, 64,
                                    lambda pb, pk, c=c, pc=pc: fw.op("act", lambda e: e.copy(out=vic[:, c, pc * 256:(pc + 1) * 256], in_=pb), reads=[pk], writes=["vic"]))
                ld("sp", vi_d[seg].rearrange("p (c v) -> p c v", c=16), vi64[:, :, :], reads=["vi64"], writes=[("vi_d", seg)])
                for pc in range(4):
                    wb, wk = load_wpiece(6144 + pc * 256)
                    for hb in range(2):
                        h = pc * 2 + hb
                        i = scnt["n"] % 2
                        scnt["n"] += 1
                        for th in range(2):
                            proj_fm(wb, wk, hb, hxT, hx_keys(128 + th * 512, 512), 128 + th * 512, 512,
                                    lambda pb, pk, th=th: fw.op("act", lambda e: e.activation(out=tf[:, th * 512:(th + 1) * 512], in_=pb, func=AF.Silu), reads=[pk], writes=["tf"]))
                        fw.op("dve", lambda e, i=i, h=h: e.tensor_scalar(out=qe_s[i][:, :], in0=tf[:, :], scalar1=normg[:, h:h + 1], scalar2=None, op0=ALU.mult),
                              reads=["tf", "normg"], writes=["st%dqe" % i])
                        ld("sp", g_d[seg][:, h * 1024:(h + 1) * 1024], qe_s[i][:, :], reads=["st%dqe" % i], writes=[("g_d", seg)])
                if seg == 0 and not cfg.get('skip_cx'):
                    with contextlib.ExitStack() as pcx:
                        Sbf_c = sb("Sbf_c", [128, 8, 128], BF16, stack=pcx)
                        Sd_c = sb("Sd_c", [128, 8, 128], stack=pcx)
                        ktok_c = sb("ktok_c", [64, 8, 128], BF16, stack=pcx)
                        for d in range(2):
                            fw.op("dve", lambda e, d=d: e.memset(S0[d][:, :, :], 0.0), writes=[("S0", d)])
                            hg_pass(d, None, kce[d], decc[d], vic, 4, S0[d], ("S0", d), Sd_c, Sbf_c, ktok_c, None, None, "cx%d" % d, "vic")
                fw.barrier()
        fw.barrier()

    if upto >= 2 and not cfg.get('skip_s2'):
      with contextlib.ExitStack() as p2:
        qeT = sb("qeT", [128, 8, T], BF16, stack=p2)
        keT = sb("keT", [128, 8, T], BF16, stack=p2)
        vi2 = sb("vi2", [64, 16, 1024], BF16, stack=p2)
        dect = sb("dect", [128, 8, 16], stack=p2)
        oF = sb("oF", [128, 8, T], BF16, stack=p2)
        gTn = sb("gTn", [128, 8, T], BF16, stack=p2)
        Sst = sb("Sst", [128, 8, 128], stack=p2)
        Sd2 = sb("Sd2", [128, 8, 128], stack=p2)
        Sbf2 = sb("Sbf2", [128, 8, 128], BF16, stack=p2)
        ktok2 = sb("ktok2", [64, 8, 128], BF16, stack=p2)
        scb2 = sb("scb2", [64, 512], BF16, stack=p2)
        osum = sb("osum", [128, 512], stack=p2)
        sq = sb("sq", [128, 512], BF16, stack=p2)
        rstd = sb("rstd", [128, 512], stack=p2)
        for d in range(2):
            fw.op("dve", lambda e, d=d: e.tensor_copy(out=Sst[:, :, :], in_=S0[d][:, :, :]), reads=[("S0", d)], writes=["Sst"])
            for seg in (range(NSEG) if d == 0 else range(NSEG - 1, -1, -1)):
                ld("sp", qeT[:, :, :], qe_d[d][seg].rearrange("p (h t) -> p h t", h=8), reads=[("qe_d", d, seg)], writes=["s2qe"])
                ld("sp", keT[:, :, :], ke_d[d][seg].rearrange("p (h t) -> p h t", h=8), reads=[("ke_d", d, seg)], writes=["s2ke"])
                ld("sp", dect[:, :, :], dec_d[d][seg].rearrange("p (h c) -> p h c", h=8), reads=[("dec_d", d, seg)], writes=["s2dec"])
                ld("sp", vi2[:, :, :], vi_d[seg].rearrange("p (c v) -> p c v", c=16), reads=[("vi_d", seg)], writes=["vi2"])
                if d == 1:
                    ld("sp", oF[:, :, :], of_d[seg].rearrange("p (h t) -> p h t", h=8), reads=[("of_d", seg)], writes=["oF"])
                    ld("sp", gTn[:, :, :], g_d[seg].rearrange("p (h t) -> p h t", h=8), reads=[("g_d", seg)], writes=["gTn"])

                def ocb(c, ob, ok, d=d):
                    cs = slice(c * 64, (c + 1) * 64)
                    obv = ob.rearrange("p (h t) -> p h t", h=8)
                    if d == 0:
                        fw.op("act", lambda e: e.copy(out=oF[:, :, cs], in_=obv), reads=[ok], writes=["oF"])
                        return
                    msb = (0 if c % 2 == 0 else 3) + 1
                    osv = osum[:, :].rearrange("p (h t) -> p h t", h=8)
                    fw.op("dve", lambda e: e.tensor_tensor(out=osv, in0=obv, in1=oF[:, :, cs], op=ALU.add), reads=[ok, "oF"], writes=["osum"])
                    fw.op("act", lambda e: e.activation(out=sq[:, :], in_=osum[:, :], func=AF.Square), reads=["osum"], writes=["sq"])
                    fw.op("pe", lambda e: e.matmul(bank(msb), lhsT=ones_b[:, :], rhs=sq[:, :], start=True, stop=True),
                          reads=["sq", "ones_b"], writes=[PSK[msb]])
                    fw.op("dve", lambda e: e.tensor_scalar(out=rstd[:, :], in0=bank(msb), scalar1=1.0 / 128.0, scalar2=NORM_EPS, op0=ALU.mult, op1=ALU.add),
                          reads=[PSK[msb]], writes=["rstd"])
                    fw.op("act", lambda e: e.activation(out=rstd[:, :], in_=rstd[:, :], func=AF.Sqrt), reads=["rstd"], writes=["rstd"])
                    fw.op("dve", lambda e: e.reciprocal(out=rstd[:, :], in_=rstd[:, :]), reads=["rstd"], writes=["rstd"])
                    fw.op("dve", lambda e: e.tensor_mul(out=osum[:, :], in0=osum[:, :], in1=rstd[:, :]), reads=["osum", "rstd"], writes=["osum"])
                    fw.op("dve", lambda e: e.tensor_tensor(out=oF[:, :, cs], in0=osv, in1=gTn[:, :, cs], op=ALU.mult),
                          reads=["osum", "gTn", "oF"], writes=["oF"])
                hg_pass(d, qeT, keT, dect, vi2, 16, Sst, "Sst", Sd2, Sbf2, ktok2, scb2, ocb, "s2", "vi2")
                dst = of_d[seg] if d == 0 else hg_d[seg]
                ld("sp", dst.rearrange("p (h t) -> p h t", h=8), oF[:, :, :], reads=["oF"],
                   writes=[("of_d", seg) if d == 0 else ("hg_d", seg)])
                if upto == 2 and d == 1 and seg == 0:
                    ld("pool", dbg.rearrange("p (h t) -> p h t", h=8), oF[:, :, :], reads=["oF"])
        fw.barrier()

    rtw = sb("rtw", [128, 2, KC, NR])
    rtb = sb("rtb", [128, 2, NR])
    for l in range(2):
        ld("sp", rtw[:, l, :, :], rtw_in[l].rearrange("(kc p) r -> p kc r", p=128), writes=["rtw"])
        ld("sp", rtb[:, l, :], rtb_in[l:l + 1, :].partition_broadcast(128), writes=["rtb"])

    def bcast_rows(dst, src_row, key):
        ld("sp", dst[:, :], src_row.partition_broadcast(128), writes=[key])

    def layer_norm(zt, zk, xn, xnk, lngb, lnbb, st, keys):
        stats, mv, rs_, nmr = st
        if cfg.get("no_ln"):
            fw.op("dve", lambda e: e.tensor_copy(out=xn[:, :], in_=zt[:, :]), reads=[zk], writes=[xnk])
            return
        for q in range(4):
            fw.op("dve", lambda e, q=q: e.bn_stats(out=stats[:, q, :], in_=zt[:, q * 512:(q + 1) * 512]), reads=[zk], writes=["lnst"])
        fw.op("dve", lambda e: e.bn_aggr(out=mv[:, :], in_=stats[:, :, :].rearrange("p a b -> p (a b)")), reads=["lnst"], writes=["lnmv"])
        fw.op("dve", lambda e: e.tensor_scalar_add(out=rs_[:, :], in0=mv[:, 1:2], scalar1=LN_EPS), reads=["lnmv"], writes=["lnrs"])
        fw.op("act", lambda e: e.activation(out=rs_[:, :], in_=rs_[:, :], func=AF.Sqrt), reads=["lnrs"], writes=["lnrs"])
        fw.op("dve", lambda e: e.reciprocal(out=rs_[:, :], in_=rs_[:, :]), reads=["lnrs"], writes=["lnrs"])
        fw.op("dve", lambda e: e.tensor_mul(out=nmr[:, :], in0=mv[:, 0:1], in1=rs_[:, 0:1]), reads=["lnmv", "lnrs"], writes=["lnnm"])
        fw.op("dve", lambda e: e.tensor_scalar(out=nmr[:, :], in0=nmr[:, :], scalar1=-1.0, scalar2=None, op0=ALU.mult), reads=["lnnm"], writes=["lnnm"])
        fw.op("act", lambda e: e.activation(out=xn[:, :], in_=zt[:, :], func=AF.Identity, scale=rs_[:, 0:1], bias=nmr[:, 0:1]),
              reads=[zk, "lnrs", "lnnm"], writes=[xnk])
        fw.op("pool", lambda e: e.tensor_mul(out=xn[:, :], in0=xn[:, :], in1=lngb[:, :]), reads=[xnk] + keys, writes=[xnk])
        fw.op("pool", lambda e: e.tensor_add(out=xn[:, :], in0=xn[:, :], in1=lnbb[:, :]), reads=[xnk] + keys, writes=[xnk])


    ys_d = [dint("ys_d%d" % s_, [128, KC * 1024], BF16) for s_ in range(NSEG)]

    def pool_mixer(seg):
        nm = "_p%d" % seg
        with contextlib.ExitStack() as pp:
            hx1 = sb("hx1" + nm, [128, KC, 1040], BF16, stack=pp)
            mixed = sb("mixed" + nm, [128, KC, T], BF16, stack=pp)
            ys = sb("ys" + nm, [128, KC, T], BF16, stack=pp)
            uT = sb("uT" + nm, [128, 1040], stack=pp)
            bA = sb("bA" + nm, [128, 1040], stack=pp)
            bB = sb("bB" + nm, [128, 1040], stack=pp)
            pinvb = sb("pinvb" + nm, [128, 4, T], stack=pp)
            xt = sb("xtp" + nm, [128, D], stack=pp)
            xh = sb("xh" + nm, [8, D], stack=pp)
            wpb = [sb("wpb%d" % i + nm, [128, KC, 256], BF16, stack=pp) for i in range(2)]
            wgb = [sb("wgb%d" % i + nm, [128, 4, 512], BF16, stack=pp) for i in range(2)]
            for g in range(4):
                ld("sp", pinvb[:, g, :], pinv_in[0:1, g * SEQL + seg * 1024: g * SEQL + (seg + 1) * 1024].partition_broadcast(128), writes=["pinvb"])
            sc_ = lambda kc: mcol(1, 16 + kc, 0, True)
            sh_ = lambda kc: mcol(1, kc, 0)
            for side, (r0, c0, ok) in enumerate(((seg * 1024 - 8, 0, seg > 0), ((seg + 1) * 1024, 1032, seg < NSEG - 1))):
                if ok:
                    ld("sp", xh[:, :], xl0_d[r0:r0 + 8, :], reads=[("xsrc", 1)], writes=["xh"])
                    trans_mod(xh, "xh", hx1, c0, sc_, sh_, "hx1", npart=8)
                else:
                    fw.op("pool", lambda e, c0=c0: e.memset(hx1[:, :, c0:c0 + 8], 0.0), writes=["hx1"])
            for tile in range(8):
                r0 = seg * 1024 + tile * 128
                ld("sp", xt[:, :], xl0_d[r0:r0 + 128, :], reads=[("xsrc", 1)], writes=["xtp"])
                trans_mod(xt, "xtp", hx1, 8 + tile * 128, sc_, sh_, "hx1")
            fw.barrier()
            pk2 = 0
            for pc in range(8):
                wb = wpb[pc % 2]
                wk = ("wpb", pc % 2)
                ld("pool", wb[:, :, :], pwin_in[:, pc * 256:(pc + 1) * 256].rearrange("(kc p) j -> p kc j", p=128), writes=[wk])
                for cb in range(2):
                    fcn = pc * 2 + cb
                    g = fcn // 4
                    for (t0, nt) in ((0, 8), (8, 512), (520, 512), (1032, 8)):
                        bk = 2 + (pk2 % 4)
                        pk2 += 1
                        mm_group(bank(bk, nt), [(wb[:, kc, cb * 128:(cb + 1) * 128], hx1[:, kc, t0:t0 + nt]) for kc in range(KC)], [wk, "hx1"], [PSK[bk]])
                        fw.op("act", lambda e, bk=bk, t0=t0, nt=nt: e.copy(out=uT[:, t0:t0 + nt], in_=bank(bk, nt)), reads=[PSK[bk]], writes=["uT"])
                    fw.op("dve", lambda e: e.tensor_add(out=bA[:, 1:1040], in0=uT[:, 0:1039], in1=uT[:, 1:1040]), reads=["uT"], writes=["bA"])
                    fin, fk = bA, "bA"
                    if g >= 1:
                        fw.op("dve", lambda e: e.tensor_add(out=bB[:, 2:1039], in0=bA[:, 1:1038], in1=bA[:, 3:1040]), reads=["bA"], writes=["bB"])
                        fin, fk = bB, "bB"
                    if g >= 2:
                        fw.op("dve", lambda e: e.tensor_add(out=bA[:, 4:1036], in0=bB[:, 2:1034], in1=bB[:, 6:1038]), reads=["bB"], writes=["bA"])
                        fin, fk = bA, "bA"
                    if g >= 3:
                        fw.op("dve", lambda e: e.tensor_add(out=bB[:, 8:1032], in0=bA[:, 4:1028], in1=bA[:, 12:1036]), reads=["bA"], writes=["bB"])
                        fin, fk = bB, "bB"
                    fw.op("pool", lambda e, fin=fin, g=g: e.tensor_mul(out=fin[:, 8:1032], in0=fin[:, 8:1032], in1=pinvb[:, g, :]), reads=[fk, "pinvb"], writes=[fk])
                    fw.op("pool", lambda e, fin=fin, fcn=fcn: e.tensor_sub(out=mixed[:, fcn, :], in0=fin[:, 8:1032], in1=uT[:, 8:1032]), reads=[fk, "uT"], writes=["mixed"])
            for g in range(4):
                wg = wgb[g % 2]
                wgk = ("wgb", g % 2)
                ld("pool", wg[:, :, :], pwgrp_in[g].rearrange("(cc p) n -> p cc n", p=128), writes=[wgk])
                for dc in range(4):
                    for th in range(2):
                        bk = 2 + (pk2 % 4)
                        pk2 += 1
                        mm_group(bank(bk), [(wg[:, cc, dc * 128:(dc + 1) * 128], mixed[:, g * 4 + cc, th * 512:(th + 1) * 512]) for cc in range(4)],
                                 [wgk, "mixed"], [PSK[bk]])
                        fw.op("act", lambda e, bk=bk, g=g, dc=dc, th=th: e.activation(
                            out=ys[:, g * 4 + dc, th * 512:(th + 1) * 512], in_=bank(bk), func=AF.Copy, scale=pscale[:, g * 4 + dc:g * 4 + dc + 1]),
                            reads=[PSK[bk], "pscale"], writes=["ys"])
            ld("sp", ys_d[seg].rearrange("p (c t) -> p c t", c=KC), ys[:, :, :], reads=["ys"], writes=[("ys_d", seg)])
            fw.barrier()

    for l in range(2):
        if upto < 3 + l:
            break
        x_src = x_in if l == 0 else xl0_d
        x_dst = xl0_d if l == 0 else out_d
        wres_in = wout_in if l == 0 else pwout_in
        for seg in range(NSEG):
            if l == 1:
                pool_mixer(seg)
            with contextlib.ExitStack() as pl:
                hT = sb("hT_%d_%d" % (l, seg), [128, KC, T], BF16, stack=pl)
                gates = sb("gates_%d_%d" % (l, seg), [128, 8, NE], stack=pl)
                with contextlib.ExitStack() as pa_:
                    nm = "_%d_%d" % (l, seg)
                    Wres = sb("Wres" + nm, [128, KC, D], BF16, stack=pa_)
                    mixT = sb("mixT" + nm, [128, KC, T], BF16, stack=pa_)
                    xt = sb("xtl" + nm, [128, D], stack=pa_)
                    zt = sb("zt" + nm, [128, D], stack=pa_)
                    gb = sb("gb" + nm, [128, D], stack=pa_)
                    lngb = sb("lngb" + nm, [128, D], stack=pa_)
                    lnbb = sb("lnbb" + nm, [128, D], stack=pa_)
                    hTf = sb("hTf" + nm, [128, KC, 128], stack=pa_)
                    stats = sb("stats" + nm, [128, 4, 6], stack=pa_)
                    mv = sb("mv" + nm, [128, 2], stack=pa_)
                    rs_ = sb("rs" + nm, [128, 1], stack=pa_)
                    nmr = sb("nmr" + nm, [128, 1], stack=pa_)
                    lg = sb("lg" + nm, [128, NR], stack=pa_)
                    rsm = sb("rsm" + nm, [128, 64], stack=pa_)
                    ee = sb("ee" + nm, [128, 8], stack=pa_)
                    top8 = sb("top8" + nm, [128, 8], stack=pa_)
                    for q in range(4):
                        ld("pool", Wres[:, q * 4:(q + 1) * 4, :], wres_in[q * 512:(q + 1) * 512, :].rearrange("(kc p) n -> p kc n", p=128), writes=["Wres"])
                    bcast_rows(gb, grow[l][0], "gb")
                    bcast_rows(lngb, ln_g[2 * l:2 * l + 1, :], "lngb")
                    bcast_rows(lnbb, ln_b[2 * l:2 * l + 1, :], "lnbb")
                    if l == 0:
                        ld("sp", mixT[:, 0:8, :], att_d[seg].rearrange("p (h t) -> p h t", h=8), reads=[("att_d", seg)], writes=["mixT"])
                        ld("sp", mixT[:, 8:16, :], hg_d[seg].rearrange("p (h t) -> p h t", h=8), reads=[("hg_d", seg)], writes=["mixT"])
                    else:
                        ld("sp", mixT[:, :, :], ys_d[seg].rearrange("p (c t) -> p c t", c=KC), reads=[("ys_d", seg)], writes=["mixT"])
                    for tile in range(8):
                        r0 = seg * 1024 + tile * 128
                        yb = 0 if tile % 2 == 0 else 4
                        ld("sp", xt[:, :], x_src[r0:r0 + 128, :], reads=[("xsrc", l)], writes=["xtl"])
                        for nb in range(4):
                            mm_group(bank(yb + nb), [(mixT[:, kc, tile * 128:(tile + 1) * 128], Wres[:, kc, nb * 512:(nb + 1) * 512]) for kc in range(KC)],
                                     ["mixT", "Wres"], [PSK[yb + nb]])
                            fw.op("dve", lambda e, nb=nb, yb=yb: e.tensor_tensor(out=zt[:, nb * 512:(nb + 1) * 512], in0=bank(yb + nb),
                                                                                 in1=gb[:, nb * 512:(nb + 1) * 512], op=ALU.mult),
                                  reads=[PSK[yb + nb], "gb"], writes=["zt"])
                        fw.op("dve", lambda e: e.tensor_scalar(out=xt[:, :], in0=xt[:, :], scalar1=ALPHA, scalar2=None, op0=ALU.mult), reads=["xtl"], writes=["xtl"])
                        fw.op("dve", lambda e: e.tensor_add(out=zt[:, :], in0=xt[:, :], in1=zt[:, :]), reads=["xtl", "zt"], writes=["zt"])
                        layer_norm(zt, "zt", xt, "xtl", lngb, lnbb, (stats, mv, rs_, nmr), ["lngb", "lnbb"])
                        ld("sp", x1_d[l][r0:r0 + 128, :], xt[:, :], reads=["xtl"], writes=[("x1_d", l)])
                    fw.barrier()
                    for tile in range(8):
                        r0 = seg * 1024 + tile * 128
                        yb = 0 if tile % 2 == 0 else 4
                        ld("sp", xt[:, :], x1_d[l][r0:r0 + 128, :], reads=[("x1_d", l)], writes=["xtl"])

                        def f32copy(kc, src, pk, eng, l=l):
                            fw.op(eng,
                                  (lambda e: e.tensor_scalar(out=hTf[:, kc, :], in0=src, scalar1=mcol(l, 64 + kc, 0, True), scalar2=mcol(l, 48 + kc, 0),
                                                             op0=ALU.mult, op1=ALU.add)) if eng == "dve" else
                                  (lambda e: e.activation(out=hTf[:, kc, :], in_=src, func=AF.Identity, scale=mcol(l, 64 + kc, 0, True), bias=mcol(l, 48 + kc, 0))),
                                  reads=[pk, "modT", "mod1"], pwrites=["hTf"])
                        if not cfg.get("no_tr"):
                            trans_mod(xt, "xtl", hT, tile * 128, lambda kc, l=l: mcol(l, 64 + kc, 0, True), lambda kc, l=l: mcol(l, 48 + kc, 0), "hT",
                                      tbanks=((2, 3) if yb == 4 else (6, 7)), f32copy=(None if cfg.get('no_f32') else f32copy))
                        if cfg.get('no_router'):
                            fw.op('dve', lambda e, tile=tile: e.memset(gates[:, tile, :], 0.25), writes=['gates'])
                        else:
                            rb_ = 1 if yb == 4 else 5
                            mm_group(bank(rb_, NR), [(hTf[:, kc, :], rtw[:, l, kc, :]) for kc in range(KC)], ["hTf", "rtw"], [PSK[rb_]])
                            fw.op("dve", lambda e, rb_=rb_, l=l: e.tensor_tensor(out=lg[:, :], in0=bank(rb_, NR), in1=rtb[:, l, :], op=ALU.add),
                                  reads=[PSK[rb_], "rtb"], writes=["lg"])
                            R_ = ["lg", "rsm"]
                            fw.op("dve", lambda e: e.reduce_max(out=rsm[:, 0:1], in_=lg[:, 0:NG], axis=AX.X), reads=["lg"], writes=["rsm"])
                            fw.op("dve", lambda e: e.tensor_scalar(out=rsm[:, 1:2], in0=rsm[:, 0:1], scalar1=-1.0, scalar2=None, op0=ALU.mult), reads=R_, writes=["rsm"])
                            fw.op("act", lambda e: e.activation(out=rsm[:, 24:24 + NG], in_=lg[:, 0:NG], func=AF.Exp, bias=rsm[:, 1:2], accum_out=rsm[:, 2:3]),
                                  reads=R_, writes=["rsm"])
                            fw.op("dve", lambda e: e.reciprocal(out=rsm[:, 3:4], in_=rsm[:, 2:3]), reads=R_, writes=["rsm"])
                            fw.op("dve", lambda e: e.tensor_scalar(out=rsm[:, 8:8 + NG], in0=lg[:, 0:NG], scalar1=rsm[:, 0:1], scalar2=None, op0=ALU.is_ge),
                                  reads=R_, writes=["rsm"])
                            for g in range(NG):
                                if g == 0:
                                    fw.op("dve", lambda e: e.tensor_scalar(out=rsm[:, 16:16 + EPG], in0=lg[:, NG:NG + EPG], scalar1=rsm[:, 8:9], scalar2=None, op0=ALU.mult),
                                          reads=R_, writes=["rsm"])
                                else:
                                    fw.op("dve", lambda e, g=g: e.scalar_tensor_tensor(out=rsm[:, 16:16 + EPG], in0=lg[:, NG + g * EPG:NG + (g + 1) * EPG],
                                                                                       scalar=rsm[:, 8 + g:9 + g], in1=rsm[:, 16:16 + EPG], op0=ALU.mult, op1=ALU.add),
                                          reads=R_, writes=["rsm"])
                            fw.op("dve", lambda e: e.reduce_max(out=rsm[:, 4:5], in_=rsm[:, 16:16 + EPG], axis=AX.X), reads=R_, writes=["rsm"])
                            fw.op("dve", lambda e: e.tensor_scalar(out=rsm[:, 5:6], in0=rsm[:, 4:5], scalar1=-1.0, scalar2=None, op0=ALU.mult), reads=R_, writes=["rsm"])
                            fw.op("dve", lambda e: e.memset(ee[:, :], 0.0), writes=["ee"])
                            fw.op("act", lambda e: e.activation(out=ee[:, 0:EPG], in_=rsm[:, 16:16 + EPG], func=AF.Exp, bias=rsm[:, 5:6]), reads=R_ + ["ee"], writes=["ee"])
                            fw.op("dve", lambda e: e.max(out=top8[:, :], in_=ee[:, :]), reads=["ee"], writes=["top8"])
                            fw.op("dve", lambda e: e.tensor_add(out=rsm[:, 6:7], in0=top8[:, 0:1], in1=top8[:, 1:2]), reads=["top8", "rsm"], writes=["rsm"])
                            fw.op("dve", lambda e: e.reciprocal(out=rsm[:, 6:7], in_=rsm[:, 6:7]), reads=R_, writes=["rsm"])
                            fw.op("dve", lambda e: e.tensor_mul(out=rsm[:, 7:8], in0=rsm[:, 6:7], in1=rsm[:, 3:4]), reads=R_, writes=["rsm"])
                            fw.op("dve", lambda e: e.scalar_tensor_tensor(out=ee[:, :], in0=ee[:, :], scalar=top8[:, 1:2], in1=ee[:, :], op0=ALU.is_ge, op1=ALU.mult),
                                  reads=["ee", "top8"], writes=["ee"])
                            fw.op("dve", lambda e: e.tensor_scalar(out=ee[:, :], in0=ee[:, :], scalar1=rsm[:, 7:8], scalar2=None, op0=ALU.mult), reads=["ee", "rsm"], writes=["ee"])
                            for g in range(NG):
                                fw.op("dve", lambda e, g=g, tile=tile: e.tensor_scalar(out=gates[:, tile, g * EPG:(g + 1) * EPG], in0=ee[:, 0:EPG],
                                                                                       scalar1=rsm[:, 8 + g:9 + g], scalar2=None, op0=ALU.mult),
                                      reads=["ee", "rsm"], writes=["gates"])
                    fw.barrier()
                with contextlib.ExitStack() as pb_:
                    nm = "_%d_%d" % (l, seg)
                    yacc = sb("yacc" + nm, [128, 8, D], stack=pb_)
                    with contextlib.ExitStack() as pb2:
                        wr = [sb("wr%d" % i + nm, [128, KC * FF], BF16, stack=pb2) for i in range(4)]
                        aT = sb("aT" + nm, [128, 4, T], BF16, stack=pb2)
                        s1 = [sb("s1_%d" % i + nm, [128, 512], stack=pb2) for i in range(2)]
                        wn = 0
                        hb_ = 0
                        ybk = 0
                        if cfg.get('no_moe'):
                            for tile in range(8):
                                fw.op('dve', lambda e, tile=tile: e.memset(yacc[:, tile, :], 0.0), writes=[('yacc', tile)])
                        for ex in (range(0) if cfg.get('no_moe') else range(NE)):
                            slots = []
                            for wi, (src, shp) in enumerate(((w1_in[l, ex], "a"), (w3_in[l, ex], "a"), (w2_in[l, ex], "b"))):
                                i = wn % 4
                                wn += 1
                                if shp == "a":
                                    for q4 in range(4):
                                        ld("pool", wr[i][:, q4 * 4 * FF:(q4 + 1) * 4 * FF].rearrange("p (kc f) -> p kc f", kc=4),
                                           src[q4 * 512:(q4 + 1) * 512, :].rearrange("(kc p) f -> p kc f", p=128), writes=[("wr", i)])
                                else:
                                    ld("pool", wr[i][:, :].rearrange("p (fc n) -> p fc n", fc=4), src.rearrange("(fc p) n -> p fc n", p=128), writes=[("wr", i)])
                                slots.append(i)
                            w1s = wr[slots[0]][:, :].rearrange("p (kc f) -> p kc f", kc=KC)
                            w3s = wr[slots[1]][:, :].rearrange("p (kc f) -> p kc f", kc=KC)
                            w2s = wr[slots[2]][:, :].rearrange("p (fc n) -> p fc n", fc=4)
                            for fc in (range(0) if cfg.get('moe_part') in ('dma', 'y') else range(4)):
                                for th in range(2):
                                    b1 = (hb_ % 2) * 2
                                    hb_ += 1
                                    mm_group(bank(b1), [(w1s[:, kc, fc * 128:(fc + 1) * 128], hT[:, kc, th * 512:(th + 1) * 512]) for kc in range(KC)],
                                             [("wr", slots[0]), "hT"], [PSK[b1]])
                                    mm_group(bank(b1 + 1), [(w3s[:, kc, fc * 128:(fc + 1) * 128], hT[:, kc, th * 512:(th + 1) * 512]) for kc in range(KC)],
                                             [("wr", slots[1]), "hT"], [PSK[b1 + 1]])
                                    si = (b1 // 2)
                                    fw.op("act", lambda e, b1=b1, si=si: e.activation(out=s1[si][:, :], in_=bank(b1), func=AF.Silu), reads=[PSK[b1]], writes=[("s1", si)])
                                    fw.op("dve", lambda e, b1=b1, si=si, fc=fc, th=th: e.tensor_tensor(out=aT[:, fc, th * 512:(th + 1) * 512], in0=bank(b1 + 1), in1=s1[si][:, :], op=ALU.mult),
                                          reads=[("s1", si), PSK[b1 + 1]], writes=["aT"])
                            for tile in (range(0) if cfg.get('moe_part') in ('dma', 'h') else range(8)):
                                for nb in range(4):
                                    yb = 4 + (ybk % 4)
                                    ybk += 1
                                    mm_group(bank(yb), [(aT[:, fc, tile * 128:(tile + 1) * 128], w2s[:, fc, nb * 512:(nb + 1) * 512]) for fc in range(4)],
                                             ["aT", ("wr", slots[2])], [PSK[yb]])
                                    if ex == 0:
                                        fw.op("dve", lambda e, yb=yb, tile=tile, nb=nb, ex=ex: e.tensor_scalar(
                                            out=yacc[:, tile, nb * 512:(nb + 1) * 512], in0=bank(yb), scalar1=gates[:, tile, ex:ex + 1], scalar2=None, op0=ALU.mult),
                                            reads=[PSK[yb], "gates"], writes=[("yacc", tile)])
                                    else:
                                        fw.op("dve", lambda e, yb=yb, tile=tile, nb=nb, ex=ex: e.scalar_tensor_tensor(
                                            out=yacc[:, tile, nb * 512:(nb + 1) * 512], in0=bank(yb), scalar=gates[:, tile, ex:ex + 1],
                                            in1=yacc[:, tile, nb * 512:(nb + 1) * 512], op0=ALU.mult, op1=ALU.add),
                                            reads=[PSK[yb], "gates"], writes=[("yacc", tile)])
                        fw.barrier()
                    with contextlib.ExitStack() as pc_:
                        xt2v = sb("xt2" + nm, [128, D], stack=pc_)
                        gb2v = sb("gb2" + nm, [128, D], stack=pc_)
                        lngb2v = sb("lngb2" + nm, [128, D], stack=pc_)
                        lnbb2v = sb("lnbb2" + nm, [128, D], stack=pc_)
                        stats2v = sb("stats2" + nm, [128, 4, 6], stack=pc_)
                        mv2v = sb("mv2" + nm, [128, 2], stack=pc_)
                        rs2v = sb("rs2" + nm, [128, 1], stack=pc_)
                        nmr2v = sb("nmr2" + nm, [128, 1], stack=pc_)
                        bcast_rows(gb2v, grow[l][1], "gb")
                        bcast_rows(lngb2v, ln_g[2 * l + 1:2 * l + 2, :], "lngb")
                        bcast_rows(lnbb2v, ln_b[2 * l + 1:2 * l + 2, :], "lnbb")
                        for tile in (range(0) if cfg.get("no_c") else range(8)):
                            r0 = seg * 1024 + tile * 128
                            ld("sp", xt2v[:, :], x1_d[l][r0:r0 + 128, :], reads=[("x1_d", l)], writes=["xtl"])
                            yt = yacc[:, tile, :]
                            fw.op("dve", lambda e, yt=yt: e.tensor_tensor(out=yt, in0=yt, in1=gb2v[:, :], op=ALU.mult), reads=[("yacc", tile), "gb"], writes=[("yacc", tile)])
                            fw.op("dve", lambda e: e.tensor_scalar(out=xt2v[:, :], in0=xt2v[:, :], scalar1=ALPHA, scalar2=None, op0=ALU.mult), reads=["xtl"], writes=["xtl"])
                            fw.op("dve", lambda e, yt=yt: e.tensor_add(out=yt, in0=xt2v[:, :], in1=yt), reads=["xtl", ("yacc", tile)], writes=[("yacc", tile)])
                            layer_norm(yt, ("yacc", tile), xt2v, "xtl", lngb2v, lnbb2v, (stats2v, mv2v, rs2v, nmr2v), ["lngb", "lnbb"])
                            ld("sp", x_dst[r0:r0 + 128, :], xt2v[:, :], reads=["xtl"], writes=[("xsrc", l + 1)])
                            if dbg is not None and upto == 3 + l:
                                ld("sp", dbg[r0:r0 + 128, :], xt2v[:, :], reads=["xtl"])
                        fw.barrier()
        fw.barrier()

    fw.barrier()
    fw.emit(es)
    es.close()
    return nc


def rope_tables(seql):
    half = 64
    n_freq = 32
    pos = np.arange(seql)
    row = (pos // GRID_W).astype(np.float32)
    col = (pos % GRID_W).astype(np.float32)
    inv = (10000.0 ** (-np.arange(n_freq, dtype=np.float32) / n_freq)).astype(np.float32)
    cos = np.zeros((128, seql + 256), np.float32)
    sin = np.zeros((128, seql + 256), np.float32)
    for d in range(128):
        hf = d // 64
        j = d % 32
        isb = (d % 64) >= 32
        ang = (row if hf == 0 else col) * inv[j]
        cos[d, 128:128 + seql] = np.cos(ang)
        sin[d, 128:128 + seql] = np.sin(ang) * (1.0 if isb else -1.0)
    R = np.zeros((128, 128), np.float32)
    for d in range(128):
        partner = d + 32 if (d % 64) < 32 else d - 32
        R[partner, d] = 1.0
    return cos, sin, R


def make_in_maps(inp, cfg):
    nseg = cfg.get("nseg", 4)
    seql = nseg * 1024
    maps = []
    cos, sin, R = rope_tables(seql)
    a = np.arange(128)
    amask = np.zeros((128, 2, 128), np.float32)
    amask[:, 0, :] = (a[:, None] >= a[None, :])
    amask[:, 1, :] = (a[:, None] <= a[None, :])
    s64 = np.arange(64)
    hmask = np.zeros((64, 2, 8, 64), np.float32)
    hmask[:, 0] = (s64[:, None] <= s64[None, :])[:, None, :]
    hmask[:, 1] = (s64[:, None] >= s64[None, :])[:, None, :]
    resetp = np.ones((128, 1024), np.float32)
    resetp[:, ::64] = 0.0
    t = np.arange(seql)
    pinv = np.zeros((4, seql), np.float32)
    for gi, w in enumerate((2, 4, 8, 16)):
        lo = np.clip(t - w // 2, 0, seql)
        hi = np.clip(t + w - w // 2, 0, seql)
        pinv[gi] = 1.0 / (hi - lo)

    def pk(v, nch):
        return np.ascontiguousarray(np.asarray(v, np.float32).reshape(nch, 128).T)
    for b in range(2):
        m = {}
        m["x"] = inp["x"][b]
        m["ctx"] = inp["ctx"][b]
        cv = np.stack([inp["c"][b], inp["c_ctx"]], axis=-1)
        m["cvec"] = cv.reshape(KC, 128, 2).transpose(1, 0, 2).reshape(128, KC * 2)
        m["ada_w"] = inp["ada_w"]
        m["ada_b"] = inp["ada_b"].reshape(2, 96, 128).transpose(2, 0, 1).reshape(128, 192)
        m["ln_g"] = inp["ln_g"].reshape(4, D)
        m["ln_b"] = inp["ln_b"].reshape(4, D)
        m["ident"] = np.eye(128, dtype=np.float32)
        m["ropeR"] = R
        m["rcos"] = cos
        m["rsin"] = sin
        m["amask"] = amask.reshape(128, 256)
        m["hmask"] = hmask.reshape(64, 1024)
        m["resetp"] = resetp
        m["mix_w_in"] = inp["mix_w_in"][0]
        m["att_sink"] = inp["att_sink"][0].reshape(1, 8)
        lb = inp["hg_lb"].reshape(2, 3, 8, 128)
        m["hg_lb"] = lb.transpose(3, 0, 1, 2).reshape(128, 48)
        m["hg_norm_g"] = pk(inp["hg_norm_g"][0], 8)
        m["mix_w_out"] = inp["mix_w_out"][0]
        m["pool_w_in"] = inp["pool_w_in"][0]
        m["pool_w_grp"] = inp["pool_w_grp"][0]
        m["pool_scale"] = pk(inp["pool_scale"][0], 16)
        m["pool_w_out"] = inp["pool_w_out"][0]
        m["pinv"] = pinv.reshape(1, 4 * seql)
        m["rt_w"] = np.concatenate([inp["rt_group_w"], inp["rt_expert_w"]], axis=-1)
        m["rt_b"] = np.concatenate([inp["rt_group_b"], inp["rt_expert_b"]], axis=-1)
        m["moe_w1"] = inp["moe_w1"]
        m["moe_w3"] = inp["moe_w3"]
        m["moe_w2"] = inp["moe_w2"]
        maps.append({k: np.ascontiguousarray(v, dtype=np.float32) for k, v in m.items()})
    return maps


def kernel(**inputs):
    inp = {k: np.asarray(v) for k, v in inputs.items()}
    cfg = dict(nseg=4, ng=4, epg=8)
    nc = build(cfg)
    maps = make_in_maps(inp, cfg)
    res = run_bass_kernel_spmd(nc, maps, core_ids=[0, 1])
    out = np.stack([np.asarray(res.results[b]["out"], dtype=np.float32) for b in range(2)], axis=0)
    return out
```
